# Optimizing a Trainium2 kernel written in Bass

```python
import math
import jax
import jax.numpy as jnp
from jax import lax
import numpy as np

D_MODEL = 1024
BATCH = 2
SEQ = 16384
DEPTH = 1

GRID_W = 64
CTX_LEN = 256
RMS_EPS = 1e-6

D_MIX = 2 * D_MODEL
HEAD_DIM = 64
SSD_WIDTH = D_MIX // 2
SSD_HEADS = SSD_WIDTH // HEAD_DIM
SSD_GROUPS = 4
SSD_HPG = SSD_HEADS // SSD_GROUPS
SSD_STATE = 128
SSD_CONV = 5
SSD_CHUNK = 128
SSD_CONV_DIM = SSD_WIDTH + 2 * SSD_GROUPS * SSD_STATE
HY_WIDTH = D_MIX - SSD_WIDTH
HY_HEADS = HY_WIDTH // HEAD_DIM
HY_ORDER = 2
HY_SHORT = 3
HY_EMB = 33
HY_BANDS = (HY_EMB - 1) // 2
HY_HIDDEN = 64
HY_TARGET = 1e-2
HY_FAST_DECAY = 0.3
HY_SLOW_DECAY = 1.5
HY_MAX_DECAY = math.log(HY_TARGET) / HY_FAST_DECAY
HY_MIN_DECAY = math.log(HY_TARGET) / HY_SLOW_DECAY
PEER_HEADS = 8
PEER_KEYS = 128
PEER_EXPERTS = PEER_KEYS * PEER_KEYS
PEER_TOPK = 16
PEER_DKEY = 256
PEER_BLOCK = 128
OFF_XBC = SSD_WIDTH
OFF_DT = OFF_XBC + SSD_CONV_DIM
OFF_HY = OFF_DT + 2 * SSD_HEADS
D_IN_PROJ = OFF_HY + (HY_ORDER + 1) * HY_WIDTH

kernel_name = 'hymba_ssd_hyena_peer_block'


def rmsnorm(x, g):
    xf = x.astype(jnp.float32)
    y = xf * lax.rsqrt(jnp.mean(xf * xf, axis=-1, keepdims=True) + RMS_EPS)
    return (y * g.astype(jnp.float32)).astype(x.dtype)


def group_rmsnorm(x, g, groups):
    shp = x.shape
    xg = x.reshape(shp[:-1] + (groups, shp[-1] // groups))
    return rmsnorm(xg, g.reshape(groups, shp[-1] // groups)).reshape(shp)


def modulate(x, shift, scale):
    return x * (1.0 + scale) + shift


def dwconv_centred(u, w, b):
    k = w.shape[0]
    y = lax.conv_general_dilated(u, w[:, None, :].astype(u.dtype), window_strides=(1,),
                                 padding=[(k // 2, k // 2)],
                                 dimension_numbers=('NWC', 'WIO', 'NWC'),
                                 feature_group_count=u.shape[-1])
    return y + b


def segsum(a):
    t = a.shape[-1]
    cs = jnp.cumsum(a, axis=-1)
    diff = cs[..., :, None] - cs[..., None, :]
    mask = jnp.tril(jnp.ones((t, t), dtype=bool))
    return jnp.where(mask, diff, -jnp.inf)


def ssd_scan(x, dt, a_head, b_mat, c_mat, h0):
    bsz, seqlen, g, r, p = x.shape
    n = b_mat.shape[-1]
    q = SSD_CHUNK
    nc = seqlen // q
    xdt = (x * dt[..., None]).reshape(bsz, nc, q, g, r, p)
    bc = b_mat.reshape(bsz, nc, q, g, n)
    cc = c_mat.reshape(bsz, nc, q, g, n)
    a = jnp.moveaxis((dt * a_head).reshape(bsz, nc, q, g, r), (3, 4), (1, 2))
    a_cum = jnp.cumsum(a, axis=-1)
    l_intra = jnp.exp(segsum(a))
    y_diag = jnp.einsum('bclgn,bcsgn,bgrcls,bcsgrp->bclgrp', cc, bc, l_intra, xdt)
    decay_to_end = jnp.exp(a_cum[..., -1:] - a_cum)
    states = jnp.einsum('bclgn,bgrcl,bclgrp->bcgrpn', bc, decay_to_end, xdt)
    states = jnp.concatenate([h0[:, None].astype(states.dtype), states], axis=1)
    chunk_a = jnp.pad(a_cum[..., -1], ((0, 0), (0, 0), (0, 0), (1, 0)))
    decay_chunk = jnp.exp(segsum(chunk_a))
    states = jnp.einsum('bgrzc,bcgrpn->bzgrpn', decay_chunk, states)
    prev_states, final_state = states[:, :-1], states[:, -1]
    y_off = jnp.einsum('bclgn,bcgrpn,bgrcl->bclgrp', cc, prev_states, jnp.exp(a_cum))
    y = (y_diag + y_off).reshape(bsz, seqlen, g, r, p)
    return y, final_state


def ssd_inputs(proj, conv_w, conv_b, dt_bias):
    bsz, l, _ = proj.shape
    z = proj[..., :OFF_XBC]
    xbc = jax.nn.silu(dwconv_centred(proj[..., OFF_XBC:OFF_DT], conv_w, conv_b))
    dt_raw = proj[..., OFF_DT:OFF_HY]
    xs = xbc[..., :SSD_WIDTH].reshape(bsz, l, SSD_GROUPS, SSD_HPG, HEAD_DIM)
    gn = SSD_GROUPS * SSD_STATE
    bm = xbc[..., SSD_WIDTH:SSD_WIDTH + gn].reshape(bsz, l, SSD_GROUPS, SSD_STATE)
    cm = xbc[..., SSD_WIDTH + gn:].reshape(bsz, l, SSD_GROUPS, SSD_STATE)
    dt_f = jax.nn.softplus(dt_raw[..., :SSD_HEADS] + dt_bias[0]).reshape(bsz, l, SSD_GROUPS, SSD_HPG)
    dt_b = jax.nn.softplus(dt_raw[..., SSD_HEADS:] + dt_bias[1]).reshape(bsz, l, SSD_GROUPS, SSD_HPG)
    return z, xs, bm, cm, dt_f, dt_b


def ssd_bidir(xs, bm, cm, dt_f, dt_b, a_f, a_b, h0_f, h0_b):
    y_f, s_f = ssd_scan(xs, dt_f, a_f, bm, cm, h0_f)
    fl = lambda t: jnp.flip(t, axis=1)
    y_b, s_b = ssd_scan(fl(xs), fl(dt_b), a_b, fl(bm), fl(cm), h0_b)
    return y_f + fl(y_b), s_f, s_b


def ssd_output(y, xs, z, d_skip, norm_g):
    bsz, l = y.shape[:2]
    y = (y + xs * d_skip.reshape(SSD_GROUPS, SSD_HPG)[:, :, None]).reshape(bsz, l, SSD_WIDTH)
    return group_rmsnorm(y * jax.nn.silu(z), norm_g, SSD_GROUPS)


def hyena_filters(seqlen, w1, b1, w2, b2, w3, sin_freq):
    f32 = jnp.float32
    t = jnp.linspace(0.0, 1.0, seqlen, dtype=f32)[:, None]
    w_ang = 2.0 * math.pi * jnp.arange(seqlen, dtype=f32) / seqlen
    bands = jnp.linspace(1e-4, HY_BANDS - 1, HY_BANDS, dtype=f32)
    ang = w_ang[:, None] * bands[None, :]
    zpos = jnp.concatenate([t, jnp.cos(ang), -jnp.sin(ang)], axis=-1)
    h = jnp.sin(sin_freq[0] * (zpos @ w1 + b1))
    h = jnp.sin(sin_freq[1] * (h @ w2 + b2))
    h = (h @ w3).astype(f32).reshape(seqlen, HY_ORDER, 2, HY_WIDTH)
    deltas = jnp.abs(jnp.linspace(HY_MIN_DECAY, HY_MAX_DECAY, HY_WIDTH, dtype=f32))
    h = h * jnp.exp(-t * deltas)[:, None, None, :]
    return jnp.moveaxis(h, 0, 2)


def long_conv_bidir(u, h_fwd, h_bwd, bias):
    seqlen = u.shape[1]
    k = jnp.concatenate([h_fwd, jnp.zeros_like(h_fwd[:1]), h_bwd[:0:-1]], axis=0)
    k_f = jnp.fft.rfft(k, axis=0)
    u_f = jnp.fft.rfft(u.astype(jnp.float32), n=2 * seqlen, axis=1)
    y = jnp.fft.irfft(u_f * k_f, n=2 * seqlen, axis=1)[:, :seqlen]
    return (y + u.astype(jnp.float32) * bias.astype(jnp.float32)).astype(u.dtype)


def raster_to_colmajor(u, rows):
    bsz, l, ch = u.shape
    return u.reshape(bsz, rows, GRID_W, ch).transpose(0, 2, 1, 3).reshape(bsz, l, ch)


def colmajor_to_raster(u, rows):
    bsz, l, ch = u.shape
    return u.reshape(bsz, GRID_W, rows, ch).transpose(0, 2, 1, 3).reshape(bsz, l, ch)


def hyena(proj_hy, short_w, short_b, filters, filt_bias, norm_g, rows):
    u = dwconv_centred(proj_hy, short_w, short_b)
    v, x1, x2 = jnp.split(u, 3, axis=-1)
    z = x1 * long_conv_bidir(v, filters[0, 0], filters[0, 1], filt_bias[0])
    if rows is None:
        z = long_conv_bidir(z, filters[1, 0], filters[1, 1], filt_bias[1])
    else:
        z = colmajor_to_raster(
            long_conv_bidir(raster_to_colmajor(z, rows), filters[1, 0], filters[1, 1], filt_bias[1]), rows)
    return group_rmsnorm(x2 * z, norm_g, HY_HEADS)


def peer(h, wq, subkeys, u_tab, v_tab):
    bsz, l, d = h.shape
    blocks = h.reshape(-1, PEER_BLOCK, d)

    def block(hb):
        q = (hb @ wq).reshape(PEER_BLOCK, PEER_HEADS, 2, PEER_DKEY // 2)
        s = jnp.einsum('thsd,snd->thsn', q, subkeys).astype(jnp.float32)
        s1, i1 = lax.top_k(s[:, :, 0], PEER_TOPK)
        s2, i2 = lax.top_k(s[:, :, 1], PEER_TOPK)
        cand = (s1[..., :, None] + s2[..., None, :]).reshape(PEER_BLOCK, PEER_HEADS, PEER_TOPK * PEER_TOPK)
        sc, ci = lax.top_k(cand, PEER_TOPK)
        e1 = jnp.take_along_axis(i1, ci // PEER_TOPK, axis=-1)
        e2 = jnp.take_along_axis(i2, ci % PEER_TOPK, axis=-1)
        idx = e1 * PEER_KEYS + e2
        gate = jax.nn.softmax(sc, axis=-1).astype(hb.dtype)
        u_sel = u_tab[idx]
        v_sel = v_tab[idx]
        act = jax.nn.gelu(jnp.einsum('td,thkd->thk', hb, u_sel), approximate=False) * gate
        return jnp.einsum('thk,thkd->td', act, v_sel)

    return lax.map(block, blocks).reshape(bsz, l, d)


def setup_inputs(seed: int = 0) -> dict:
    key = jax.random.key(seed)
    ks = jax.random.split(key, 32)
    f32 = jnp.float32

    def nrm(k, shape, scale):
        return jax.random.normal(k, shape, f32) * scale

    def gain(k, shape):
        return 1.0 + 0.02 * jax.random.normal(k, shape, f32)

    dt0 = jnp.exp(jax.random.uniform(ks[9], (DEPTH, 2, SSD_HEADS), f32, math.log(1e-3), math.log(1e-1)))
    dt_bias = dt0 + jnp.log(-jnp.expm1(-dt0))
    a_log = jnp.log(jax.random.uniform(ks[8], (DEPTH, 2, SSD_HEADS), f32, 1.0, 16.0))
    return {
        'x': nrm(ks[0], (BATCH, SEQ, D_MODEL), 1.0),
        'c': nrm(ks[1], (BATCH, D_MODEL), 1.0),
        'ctx': nrm(ks[2], (BATCH, CTX_LEN, D_MODEL), 1.0),
        'c_ctx': nrm(ks[3], (D_MODEL,), 1.0),
        'w_mod': nrm(ks[4], (DEPTH, D_MODEL, 6 * D_MODEL), 0.5 * D_MODEL ** -0.5),
        'b_mod': nrm(ks[5], (DEPTH, 6 * D_MODEL), 0.02),
        'norm1': gain(ks[6], (DEPTH, D_MODEL)),
        'w_in': nrm(ks[7], (DEPTH, D_MODEL, D_IN_PROJ), D_MODEL ** -0.5),
        'ssd_conv_w': nrm(ks[10], (DEPTH, SSD_CONV, SSD_CONV_DIM), SSD_CONV ** -0.5),
        'ssd_conv_b': nrm(ks[11], (DEPTH, SSD_CONV_DIM), 0.02),
        'ssd_a_log': a_log,
        'ssd_dt_bias': dt_bias,
        'ssd_d': gain(ks[12], (DEPTH, SSD_HEADS)),
        'ssd_norm': gain(ks[13], (DEPTH, SSD_WIDTH)),
        'hy_short_w': nrm(ks[14], (DEPTH, HY_SHORT, (HY_ORDER + 1) * HY_WIDTH), HY_SHORT ** -0.5),
        'hy_short_b': nrm(ks[15], (DEPTH, (HY_ORDER + 1) * HY_WIDTH), 0.02),
        'hy_w1': nrm(ks[16], (DEPTH, HY_EMB, HY_HIDDEN), HY_EMB ** -0.5),
        'hy_b1': nrm(ks[17], (DEPTH, HY_HIDDEN), 0.02),
        'hy_w2': nrm(ks[18], (DEPTH, HY_HIDDEN, HY_HIDDEN), HY_HIDDEN ** -0.5),
        'hy_b2': nrm(ks[19], (DEPTH, HY_HIDDEN), 0.02),
        'hy_w3': nrm(ks[20], (DEPTH, HY_HIDDEN, HY_ORDER * 2 * HY_WIDTH), 0.05 * HY_HIDDEN ** -0.5),
        'hy_sin_freq': 1.0 + 0.1 * jax.random.normal(ks[21], (DEPTH, 2, HY_HIDDEN), f32),
        'hy_filt_bias': nrm(ks[22], (DEPTH, HY_ORDER, HY_WIDTH), 0.5),
        'hy_norm': gain(ks[23], (DEPTH, HY_WIDTH)),
        'w_out': nrm(ks[24], (DEPTH, D_MIX, D_MODEL), D_MIX ** -0.5),
        'norm2': gain(ks[25], (DEPTH, D_MODEL)),
        'peer_wq': nrm(ks[26], (DEPTH, D_MODEL, PEER_HEADS * PEER_DKEY), D_MODEL ** -0.5),
        'peer_subkeys': nrm(ks[27], (DEPTH, 2, PEER_KEYS, PEER_DKEY // 2), (PEER_DKEY // 2) ** -0.5),
        'peer_u': nrm(ks[28], (DEPTH, PEER_EXPERTS, D_MODEL), D_MODEL ** -0.5),
        'peer_v': nrm(ks[29], (DEPTH, PEER_EXPERTS, D_MODEL), 1.0),
        'final_norm': gain(ks[30], (D_MODEL,)),
    }


def reference(x, c, ctx, c_ctx, w_mod, b_mod, norm1, w_in, ssd_conv_w, ssd_conv_b, ssd_a_log,
              ssd_dt_bias, ssd_d, ssd_norm, hy_short_w, hy_short_b, hy_w1, hy_b1, hy_w2, hy_b2,
              hy_w3, hy_sin_freq, hy_filt_bias, hy_norm, w_out, norm2, peer_wq, peer_subkeys,
              peer_u, peer_v, final_norm):
    rows = x.shape[1] // GRID_W
    h_lat = x
    h_ctx = ctx
    bsz = x.shape[0]
    for i in range(DEPTH):
        last = i == DEPTH - 1
        mod_l = (jax.nn.silu(c) @ w_mod[i] + b_mod[i])[:, None, :]
        mod_c = (jax.nn.silu(c_ctx) @ w_mod[i] + b_mod[i])[None, None, :]
        sh1_l, sc1_l, g1_l, sh2_l, sc2_l, g2_l = jnp.split(mod_l, 6, axis=-1)
        sh1_c, sc1_c, g1_c, sh2_c, sc2_c, g2_c = jnp.split(mod_c, 6, axis=-1)

        p_l = modulate(rmsnorm(h_lat, norm1[i]), sh1_l, sc1_l) @ w_in[i]
        p_c = modulate(rmsnorm(h_ctx, norm1[i]), sh1_c, sc1_c) @ w_in[i]

        a_f = -jnp.exp(ssd_a_log[i, 0]).reshape(SSD_GROUPS, SSD_HPG)
        a_b = -jnp.exp(ssd_a_log[i, 1]).reshape(SSD_GROUPS, SSD_HPG)
        z_c, xs_c, b_c, c_c, dtf_c, dtb_c = ssd_inputs(p_c, ssd_conv_w[i], ssd_conv_b[i], ssd_dt_bias[i])
        z_l, xs_l, b_l, c_l, dtf_l, dtb_l = ssd_inputs(p_l, ssd_conv_w[i], ssd_conv_b[i], ssd_dt_bias[i])
        h0 = jnp.zeros((bsz, SSD_GROUPS, SSD_HPG, HEAD_DIM, SSD_STATE), dtype=xs_c.dtype)
        y_c, s_f, s_b = ssd_bidir(xs_c, b_c, c_c, dtf_c, dtb_c, a_f, a_b, h0, h0)
        y_l, _, _ = ssd_bidir(xs_l, b_l, c_l, dtf_l, dtb_l, a_f, a_b, s_f, s_b)
        o_ssd_l = ssd_output(y_l, xs_l, z_l, ssd_d[i], ssd_norm[i])

        filt_l = hyena_filters(h_lat.shape[1], hy_w1[i], hy_b1[i], hy_w2[i], hy_b2[i], hy_w3[i], hy_sin_freq[i])
        o_hy_l = hyena(p_l[..., OFF_HY:], hy_short_w[i], hy_short_b[i], filt_l, hy_filt_bias[i], hy_norm[i], rows)

        mix_l = jnp.concatenate([o_ssd_l, o_hy_l], axis=-1) @ w_out[i]
        h_lat = h_lat + g1_l * mix_l

        f_l = modulate(rmsnorm(h_lat, norm2[i]), sh2_l, sc2_l)
        h_lat = h_lat + g2_l * peer(f_l, peer_wq[i], peer_subkeys[i], peer_u[i], peer_v[i])

        if not last:
            o_ssd_c = ssd_output(y_c, xs_c, z_c, ssd_d[i], ssd_norm[i])
            filt_c = hyena_filters(h_ctx.shape[1], hy_w1[i], hy_b1[i], hy_w2[i], hy_b2[i], hy_w3[i], hy_sin_freq[i])
            o_hy_c = hyena(p_c[..., OFF_HY:], hy_short_w[i], hy_short_b[i], filt_c, hy_filt_bias[i], hy_norm[i], None)
            h_ctx = h_ctx + g1_c * (jnp.concatenate([o_ssd_c, o_hy_c], axis=-1) @ w_out[i])
            f_c = modulate(rmsnorm(h_ctx, norm2[i]), sh2_c, sc2_c)
            h_ctx = h_ctx + g2_c * peer(f_c, peer_wq[i], peer_subkeys[i], peer_u[i], peer_v[i])
    return rmsnorm(h_lat, final_norm)
```

```python
import math
import os
from contextlib import ExitStack
import numpy as np
import concourse.bass as bass
import concourse.mybir as mybir
from concourse.bass_utils import run_bass_kernel_spmd

F32 = mybir.dt.float32
BF16 = mybir.dt.bfloat16
U32 = mybir.dt.uint32
AF = mybir.ActivationFunctionType
ALU = mybir.AluOpType
AX = mybir.AxisListType

D = 1024
L = 16384
CTX = 256
NCORE = 8
EPS = 1e-6
OFF_XBC = 1024
OFF_DT = 3072
OFF_HY = 3104
NWC = 9 * 128 + 8
NCOLV = 70
NCH = 130


class T:
    __slots__ = ("ap", "w", "r", "name", "excl")

    def __init__(self, ap, name="", excl=False):
        self.ap = ap
        self.w = None
        self.r = {}
        self.name = name
        self.excl = excl

    def __getitem__(self, idx):
        return V(self, self.ap[idx])

    def v(self, ap):
        return V(self, ap)


class V:
    __slots__ = ("t", "ap")

    def __init__(self, t, ap):
        self.t = t
        self.ap = ap

    def __getitem__(self, idx):
        return V(self.t, self.ap[idx])


def _ap(x):
    return x.ap if isinstance(x, (V, T)) else x


def _t(x):
    return x.t if isinstance(x, V) else x


class Sched:
    ENG = ("pe", "act", "dve", "pool", "sp")
    NDMA = 40

    def __init__(self, nc, stack):
        self.nc = nc
        self.e = {"pe": nc.tensor, "act": nc.scalar, "dve": nc.vector, "pool": nc.gpsimd, "sp": nc.sync}
        self.sem = {}
        self.cnt = {}
        for k in self.ENG:
            self.sem[k] = stack.enter_context(nc.semaphore("s_" + k))
            self.cnt[k] = 0
        for i in range(self.NDMA):
            k = "d%d" % i
            self.sem[k] = stack.enter_context(nc.semaphore("s_" + k))
            self.cnt[k] = 0
        self.sem["cc"] = stack.enter_context(nc.semaphore("s_cc"))
        self.cnt["cc"] = 0
        self.waited = {k: {} for k in self.ENG}
        self.dma_rr = 0

    def _deps(self, reads, writes):
        deps = {}

        def add(k, v):
            if deps.get(k, 0) < v:
                deps[k] = v
        for t in reads:
            if t.w is not None:
                add(*t.w)
            if t.excl:
                for k, v in t.r.items():
                    add(k, v)
        for t in writes:
            if t.w is not None:
                add(*t.w)
            for k, v in t.r.items():
                add(k, v)
        return deps

    def _wait(self, eng, deps):
        for k, v in deps.items():
            if k == eng and eng == "pe":
                continue
            if self.waited[eng].get(k, 0) >= v:
                continue
            self.e[eng].wait_ge(self.sem[k], v)
            self.waited[eng][k] = v

    def _mark(self, tok, reads, writes):
        for t in reads:
            if t.r.get(tok[0], 0) < tok[1]:
                t.r[tok[0]] = tok[1]
        for t in writes:
            t.w = tok
            t.r = {}

    def op(self, eng, fn, r=(), w=(), inc=True):
        reads = [_t(x) for x in r]
        writes = [_t(x) for x in w]
        self._wait(eng, self._deps(reads, writes))
        ins = fn(self.e[eng])
        if inc:
            ins.then_inc(self.sem[eng], 1)
            self.cnt[eng] += 1
            tok = (eng, self.cnt[eng])
        else:
            tok = (eng, self.cnt[eng] + 1)
        self._mark(tok, reads, writes)
        return ins

    def dma(self, out, in_, q="sp", **kw):
        reads = [_t(in_)]
        writes = [_t(out)]
        deps = self._deps(reads, writes)
        k = "d%d" % self.dma_rr
        self.dma_rr = (self.dma_rr + 1) % self.NDMA
        if self.cnt[k] > 0 and deps.get(k, 0) < self.cnt[k]:
            deps[k] = self.cnt[k]
        self._wait(q, deps)
        ins = self.e[q].dma_start(out=_ap(out), in_=_ap(in_), **kw)
        ins.then_inc(self.sem[k], 16)
        self.cnt[k] += 16
        self._mark((k, self.cnt[k]), reads, writes)
        return ins

    def finish(self, tiles, eng="sp"):
        deps = self._deps([_t(x) for x in tiles], [])
        for k in self.ENG:
            if k != eng and self.cnt[k] > 0:
                deps[k] = max(deps.get(k, 0), self.cnt[k])
        for k in self.sem:
            if k.startswith("d") and self.cnt[k] > 0:
                deps[k] = max(deps.get(k, 0), self.cnt[k])
        self._wait(eng, deps)


class Ctx:
    pass


def build_nc(dbg=None):
    nc = bass.Bass("TRN2", target_bir_lowering=False)
    g = Ctx()
    g.nc = nc
    st = ExitStack()
    g.st = st
    S = Sched(nc, st)
    g.S = S

    def dram_in(name, shape, dt=F32):
        return T(nc.dram_tensor(name, list(shape), dt, kind="ExternalInput").ap(), name)

    def dram_out(name, shape, dt=F32):
        return T(nc.dram_tensor(name, list(shape), dt, kind="ExternalOutput").ap(), name)

    def dram_tmp(name, shape, dt):
        return T(nc.dram_tensor(name, list(shape), dt, kind="Internal").ap(), name)

    g.cur = st

    def sb(name, shape, dt):
        return T(g.cur.enter_context(nc.sbuf_tensor("sb_" + name, list(shape), dt))[:], name)

    g.sb = sb
    g.dram_tmp = dram_tmp

    def sb_main(name, shape, dt):
        return T(st.enter_context(nc.sbuf_tensor("sb_" + name, list(shape), dt))[:], name)

    g.sb_main = sb_main
    I = Ctx()
    g.I = I
    I.x_own = dram_in("x_own", [L, D])
    I.x_oth = dram_in("x_oth", [L, D])
    I.ctx_own = dram_in("ctx_own", [CTX, D])
    I.cT = dram_in("cT", [D, 3])
    I.w_mod = dram_in("w_mod", [D, 6 * D])
    I.b_mod = dram_in("b_mod", [6 * D])
    I.colv = dram_in("colv", [128, NCOLV])
    I.w_k = dram_in("w_k", [D, NWC])
    I.ident = dram_in("ident", [128, 128])
    I.rowv = dram_in("rowv", [1, 384])
    I.cst = dram_in("cst", [128, 4, 128])
    I.cst2 = dram_in("cst2", [128, 2, 128])
    I.hycol = dram_in("hycol", [128, NHC])
    I.hyrow = dram_in("hyrow", [1, 128])
    I.w3k = dram_in("w3k", [64, 4, 128])
    I.hy64 = dram_in("hy64", [64, 4])
    I.hy_w1 = dram_in("hy_w1", [33, 64])
    I.hy_w2 = dram_in("hy_w2", [64, 64])
    I.zA = dram_in("zA", [33, L])
    I.fftc = dram_in("fftc", [128, NFC])
    I.zB = dram_in("zB", [33, L])
    I.x_res = dram_in("x_res", [4096, D])
    I.final_norm = dram_in("final_norm", [D])
    I.norm2 = dram_in("norm2", [D])
    I.w_out = dram_in("w_out", [2 * D, D])
    I.wqT = dram_in("wqT", [2048, D])
    I.skT = dram_in("skT", [128, 2, 128])
    I.u2 = dram_in("u2", [D, 128, 128])
    I.v2 = dram_in("v2", [128, 128, D])
    I.pbcol = dram_in("pbcol", [128, NPB])
    I.iota = dram_in("iota", [128, 128])
    g.ps = [T(st.enter_context(nc.psum_tensor("ps%d" % i, [128, 512], F32))[:], "ps%d" % i, excl=True) for i in range(8)]
    g.ident = sb("ident", [128, 128], F32)
    S.dma(g.ident, I.ident)
    g.identb = sb("identb", [128, 128], BF16)
    S.op("dve", lambda e: e.tensor_copy(out=g.identb.ap, in_=g.ident.ap), r=[g.ident], w=[g.identb])
    g.ones_ms = sb("ones_ms", [128, 128], BF16)
    S.op("dve", lambda e: e.memset(g.ones_ms.ap, 1.0 / D), w=[g.ones_ms])
    g.epsb = sb("epsb", [128, 1], F32)
    S.op("dve", lambda e: e.memset(g.epsb.ap, EPS), w=[g.epsb])

    O = Ctx()
    g.O = O
    phase_mod(g)
    if dbg == "mod":
        O.dbg_g1 = dram_out("dbg_g1", [128, 8, 3], F32)
        O.dbg_modc = dram_out("dbg_modc", [128, 16, 3], F32)
        S.dma(O.dbg_g1, g.G1)
        S.dma(O.dbg_modc, g.modc)
        S.finish([O.dbg_g1, O.dbg_modc])
        st.close()
        return nc
    g.dt_raw = sb("dt_raw", [128, 130, 8], F32)
    g.PH = g.dram_tmp("PH", [3, 128, 2, L], BF16)
    g.PS = g.dram_tmp("PS", [6, 128, L], BF16)
    g.PC = g.dram_tmp("PC", [4, 128, CTX], BF16)
    if dbg not in ("cc", "b", "b1c"):
        with ExitStack() as sc:
            g.cur = sc
            phase_a1(g, dbg)
            barrier(g)
    g.cur = st
    g.XIN = g.dram_tmp("XIN", [512, L], BF16)
    g.XG = g.dram_tmp("XG", [512 * NCORE, L], BF16)
    g.OSSD = T(g.XIN.ap[0:256].rearrange("(b c) t -> b c t", c=128), "OSSD")
    g.OHY = T(g.XIN.ap[256:512].rearrange("(s c) t -> c s t", c=128), "OHY")
    if dbg is not None and dbg.startswith("hy"):
        with ExitStack() as sc:
            g.cur = sc
            UT = phase_hyena(g, dbg)
            if dbg in ("hy_a", "hy_b"):
                O.dbg_ut = dram_out("dbg_ut", [128, 128, 256], BF16)
                S.dma(O.dbg_ut, UT)
                O.dbg_kx = dram_out("dbg_kx", [2, 128, KXW], BF16)
                for o in range(2):
                    S.dma(O.dbg_kx[o], g.KXR[o])
            else:
                O.dbg_ohy = dram_out("dbg_ohy", [128, 2, L], BF16)
                S.dma(O.dbg_ohy, g.OHY)
            S.finish([])
        st.close()
        return nc
    if dbg == "ssd":
        with ExitStack() as sc:
            g.cur = sc
            phase_ssd(g, dbg)
            nch = int(os.environ.get("SSD_NCH", "4"))
            O.dbg_ossd = dram_out("dbg_ossd", [2, 128, nch * 128], BF16)
            S.dma(O.dbg_ossd, g.OSSD[:, :, 0:nch * 128])
            S.finish([O.dbg_ossd])
        st.close()
        return nc
    def alloc_b_persist():
        g.iota = sb_main("iota", [128, 128], F32)
        S.dma(g.iota, I.iota)
        g.modr = sb_main("modr", [128, 4, D], F32)
        g.fnr = sb_main("fnr", [128, D], F32)

    if dbg in ("cc", "b", "b1c"):
        with ExitStack() as sc:
            g.cur = sc
            zt_ = sb("zfill", [128, L], BF16)
            S.op("dve", lambda e: e.memset(zt_.ap, 0.0), w=[zt_])
            xin_t = T(g.XIN.ap, "XINall")
            for j in range(4):
                S.dma(xin_t[j * 128:(j + 1) * 128, :], zt_, q="sp")
            if dbg == "b1c":
                for j in range(32):
                    S.dma(g.XG[j * 128:(j + 1) * 128, :], zt_, q=("sp" if j % 2 else "pool"))
            else:
                collective_allgather(g, g.XIN.ap, g.XG, [xin_t])
            barrier(g)
        g.cur = st
        alloc_b_persist()
        if dbg == "cc":
            O.dbg_xg = dram_out("dbg_xg", [512 * NCORE, 64], BF16)
            S.dma(O.dbg_xg, g.XG[:, 0:64])
            S.finish([])
        else:
            with ExitStack() as sc:
                g.cur = sc
                phase_b1(g)
            with ExitStack() as sc:
                g.cur = sc
                phase_b2(g)
        st.close()
        return nc
    if dbg is None:
        with ExitStack() as sc:
            g.cur = sc
            phase_ssd(g, None)
            barrier(g)
        with ExitStack() as sc:
            g.cur = sc
            phase_hyena(g, None)
            barrier(g)
        collective_allgather(g, g.XIN.ap, g.XG, [g.OSSD, g.OHY])
        barrier(g)
        g.cur = st
        alloc_b_persist()
        with ExitStack() as sc:
            g.cur = sc
            phase_b1(g)
        with ExitStack() as sc:
            g.cur = sc
            phase_b2(g)
        st.close()
        return nc
    if dbg == "a1":
        O.dbg_ph = dram_out("dbg_ph", [3, 128, 2, 1024], BF16)
        O.dbg_ps = dram_out("dbg_ps", [6, 128, 1024], BF16)
        O.dbg_pc = dram_out("dbg_pc", [4, 128, CTX], BF16)
        O.dbg_dt = dram_out("dbg_dt", [128, 130, 8], F32)
        if g.lvl >= 9:
            for j in range(3):
                for b in range(2):
                    S.dma(O.dbg_ph[j, :, b, :], g.PH[j, :, b, 0:1024], q="sp")
            for j in range(6):
                S.dma(O.dbg_ps[j], g.PS[j, :, 0:1024], q="sp")
        if g.lvl >= 5:
            for j in range(4):
                S.dma(O.dbg_pc[j], g.PC[j], q="sp")
        if os.environ.get("NODT") is None:
            S.dma(O.dbg_dt, g.dt_raw, q="sp")
        print("sbuf remaining", nc.sbuf_bytes_remaining)
        S.finish([O.dbg_ph, O.dbg_ps, O.dbg_pc, O.dbg_dt])
    st.close()
    return nc


def phase_mod(g):
    S, I, sb, ps = g.S, g.I, g.sb, g.ps
    cT = sb("cT", [128, 8, 3], F32)
    S.dma(cT, I.cT.v(I.cT.ap.rearrange("(kc p) j -> p kc j", p=128)))
    scT = sb("scT", [128, 8, 3], F32)
    S.op("act", lambda e: e.activation(out=scT.ap, in_=cT.ap, func=AF.Silu), r=[cT], w=[scT])
    g.scT = scT
    colv = sb("colv", [128, NCOLV], F32)
    S.dma(colv, I.colv)
    g.colv = colv
    bm = colv[:, 0:16]
    n1 = colv[:, 16:24]
    modc = sb("modc", [128, 16, 3], F32)
    G1 = sb("G1", [128, 8, 3], F32)
    st_outer = g.cur
    sc_tmp = ExitStack()
    g.cur = sc_tmp
    wm = [sb("wm%d" % i, [128, 8, 512], F32) for i in range(2)]
    for cb in range(4):
        w = wm[cb % 2]
        S.dma(w, I.w_mod.v(I.w_mod.ap[:, cb * 512:(cb + 1) * 512].rearrange("(kc p) n -> p kc n", p=128)),
              q="sp" if cb % 2 == 0 else "pool")
        p = ps[cb % 2]
        for j in range(4):
            for kc in range(8):
                S.op("pe", lambda e: e.matmul(p.ap[:, j * 4:j * 4 + 3], lhsT=w.ap[:, kc, j * 128:(j + 1) * 128],
                                              rhs=scT.ap[:, kc, :], start=(kc == 0), stop=(kc == 7)),
                     r=[w, scT], w=[p], inc=(kc == 7))
        S.op("dve", lambda e: e.tensor_copy(out=modc.ap[:, cb * 4:(cb + 1) * 4, :],
                                            in_=p.ap[:, 0:16].rearrange("p (j q) -> p j q", q=4)[:, :, 0:3]),
             r=[p], w=[modc])
    S.op("dve", lambda e: e.tensor_tensor(out=modc.ap, in0=modc.ap, in1=bm.ap.unsqueeze(2).to_broadcast([128, 16, 3]),
                                          op=ALU.add), r=[modc, bm], w=[modc])
    S.op("dve", lambda e: e.tensor_scalar(out=G1.ap, in0=modc.ap[:, 8:16, :], scalar1=1.0, scalar2=None, op0=ALU.add),
         r=[modc], w=[G1])
    S.op("dve", lambda e: e.tensor_tensor(out=G1.ap, in0=G1.ap, in1=n1.ap.unsqueeze(2).to_broadcast([128, 8, 3]),
                                          op=ALU.mult), r=[G1, n1], w=[G1])
    g.G1 = G1
    g.modc = modc
    barrier(g)
    sc_tmp.close()
    g.cur = st_outer


def phase_a1(g, dbg):
    S, I, sb, ps, nc = g.S, g.I, g.sb, g.ps, g.nc
    if dbg is not None:
        S.op("pool", lambda e: e.memset(g.dt_raw.ap, 0.0), w=[g.dt_raw])
    wk = sb("wk", [128, 8, NWC], BF16)
    import os
    if os.environ.get("NOWK") is None:
        for kc in range(8):
            S.dma(wk[:, kc, :], I.w_k[kc * 128:(kc + 1) * 128, :], q="pool")
    xb = [sb("xb%d" % i, [128, 4, D], F32) for i in range(2)]
    raw = [[sb("raw%d_%d" % (i, kc), [128, 512], BF16) for kc in range(8)] for i in range(2)]
    sq = [[sb("sq%d_%d" % (i, kc), [128, 512], BF16) for kc in range(8)] for i in range(2)]
    tmp = [[sb("tmp%d_%d" % (i, kc), [128, 512], BF16) for kc in range(8)] for i in range(2)]
    xm = [[sb("xm%d_%d" % (i, kc), [128, 512], BF16) for kc in range(8)] for i in range(2)]
    rstd = [sb("rstd%d" % i, [128, 512], F32) for i in range(2)]
    ob = [sb("ob%d" % i, [128, 512], BF16) for i in range(4)]
    cnt = {"b": 0, "o": 0, "p": 0}

    def block(src_ap, ntok, mv, outs, dt_chunk0):
        i = cnt["b"] % 2
        cnt["b"] += 1
        nth = ntok // 128
        if os.environ.get("NOBLK") is not None:
            return
        S.dma(xb[i][:, 0:nth, :], V(src_ap[0], src_ap[1].rearrange("(th p) d -> p th d", p=128)), q="sp")
        bs = os.environ.get("BS", "z")
        if bs == "a":
            return
        for kc in range(8):
            p = ps[kc % 3]
            for th in range(nth):
                S.op("pe", lambda e: e.transpose(out=p.ap[:, th * 128:(th + 1) * 128],
                                                 in_=xb[i].ap[:, th, kc * 128:(kc + 1) * 128], identity=g.ident.ap),
                     r=[xb[i], g.ident], w=[p], inc=(th == nth - 1))
            if bs == "b":
                continue
            S.op("dve", lambda e: e.tensor_copy(out=raw[i][kc].ap[:, 0:ntok], in_=p.ap[:, 0:ntok]), r=[p], w=[raw[i][kc]])
            if bs == "c":
                continue
            if bs == "d":
                S.op("act", lambda e: e.copy(out=sq[i][kc].ap[:, 0:ntok], in_=p.ap[:, 0:ntok]), r=[p], w=[sq[i][kc]])
            elif bs == "e":
                S.op("act", lambda e: e.activation(out=sq[i][kc].ap[:, 0:ntok], in_=raw[i][kc].ap[:, 0:ntok], func=AF.Square),
                     r=[raw[i][kc]], w=[sq[i][kc]])
            else:
                S.op("act", lambda e: e.activation(out=sq[i][kc].ap[:, 0:ntok], in_=p.ap[:, 0:ntok], func=AF.Square),
                     r=[p], w=[sq[i][kc]])
        if g.lvl < 2:
            return
        pss = ps[3]
        for kc in range(8):
            S.op("pe", lambda e: e.matmul(pss.ap[:, 0:ntok], lhsT=g.ones_ms.ap, rhs=sq[i][kc].ap[:, 0:ntok],
                                          start=(kc == 0), stop=(kc == 7)), r=[g.ones_ms, sq[i][kc]], w=[pss], inc=(kc == 7))
        S.op("act", lambda e: e.activation(out=rstd[i].ap[:, 0:ntok], in_=pss.ap[:, 0:ntok], func=AF.Ln,
                                           bias=g.epsb.ap[:, 0:1], scale=1.0), r=[pss, g.epsb], w=[rstd[i]])
        S.op("act", lambda e: e.activation(out=rstd[i].ap[:, 0:ntok], in_=rstd[i].ap[:, 0:ntok], func=AF.Exp, scale=-0.5),
             r=[rstd[i]], w=[rstd[i]])
        if g.lvl < 3:
            return
        for kc in range(8):
            S.op("dve", lambda e: e.scalar_tensor_tensor(out=tmp[i][kc].ap[:, 0:ntok], in0=raw[i][kc].ap[:, 0:ntok],
                                                         scalar=g.G1.ap[:, kc, mv:mv + 1], in1=rstd[i].ap[:, 0:ntok],
                                                         op0=ALU.mult, op1=ALU.mult),
                 r=[raw[i][kc], g.G1, rstd[i]], w=[tmp[i][kc]])
            S.op("act", lambda e: e.activation(out=xm[i][kc].ap[:, 0:ntok], in_=tmp[i][kc].ap[:, 0:ntok], func=AF.Identity,
                                               bias=g.modc.ap[:, kc, mv:mv + 1], scale=1.0),
                 r=[tmp[i][kc], g.modc], w=[xm[i][kc]])
        if g.lvl < 4:
            return
        for (wc, dest) in outs:
            po = ps[4 + cnt["p"] % 4]
            cnt["p"] += 1
            for kc in range(8):
                S.op("pe", lambda e: e.matmul(po.ap[:, 0:ntok], lhsT=wk.ap[:, kc, wc * 128:(wc + 1) * 128],
                                              rhs=xm[i][kc].ap[:, 0:ntok], start=(kc == 0), stop=(kc == 7)),
                     r=[wk, xm[i][kc]], w=[po], inc=(kc == 7))
            o = ob[cnt["o"] % 4]
            cnt["o"] += 1
            if cnt["o"] % 2 == 0:
                S.op("act", lambda e: e.copy(out=o.ap[:, 0:ntok], in_=po.ap[:, 0:ntok]), r=[po], w=[o])
            else:
                S.op("dve", lambda e: e.tensor_copy(out=o.ap[:, 0:ntok], in_=po.ap[:, 0:ntok]), r=[po], w=[o])
            S.dma(dest, o[:, 0:ntok], q="pool")
        if dt_chunk0 is not None and g.lvl >= 6:
            po = ps[4 + cnt["p"] % 4]
            cnt["p"] += 1
            for th in range(nth):
                for kc in range(8):
                    S.op("pe", lambda e: e.matmul(po.ap[:, th * 8:(th + 1) * 8], lhsT=xm[i][kc].ap[:, th * 128:(th + 1) * 128],
                                                  rhs=wk.ap[:, kc, 9 * 128:9 * 128 + 8], start=(kc == 0), stop=(kc == 7)),
                         r=[wk, xm[i][kc]], w=[po], inc=(kc == 7))
            S.op("dve", lambda e: e.tensor_copy(out=g.dt_raw.ap[:, dt_chunk0:dt_chunk0 + nth, :],
                                                in_=po.ap[:, 0:nth * 8].rearrange("p (c q) -> p c q", q=8)),
                 r=[po], w=[g.dt_raw])

    nblk = L // 512
    if dbg is not None:
        nblk = int(os.environ.get("A1_NBLK", "2"))
    import os
    g.lvl = int(os.environ.get("A1LVL", "9"))
    if g.lvl < 9:
        nblk = 0
    block((I.ctx_own, I.ctx_own.ap), CTX, 2, [(5 + j, g.PC[j]) for j in range(4)], 128)
    for bi in range(nblk):
        t0 = bi * 512
        outs = [(j, g.PH[j, :, 0, t0:t0 + 512]) for j in range(3)]
        outs += [(3 + j, g.PS[j, :, t0:t0 + 512]) for j in range(6)]
        block((I.x_own, I.x_own.ap[t0:t0 + 512, :]), 512, 0, outs, bi * 4)
    for bi in range(nblk):
        t0 = bi * 512
        outs = [(j, g.PH[j, :, 1, t0:t0 + 512]) for j in range(3)]
        block((I.x_oth, I.x_oth.ap[t0:t0 + 512, :]), 512, 1, outs, None)


def _cst():
    k = np.arange(128)[:, None]
    i = np.arange(128)[None, :]
    tri = (k <= i).astype(np.float32)
    triu = (k >= i).astype(np.float32)
    negf = np.where(i >= k, 0.0, -30000.0).astype(np.float32)
    negb = np.where(i <= k, 0.0, -30000.0).astype(np.float32)
    return np.ascontiguousarray(np.stack([tri, triu, negf, negb], axis=1))


CST = _cst()


def _hy_consts():
    t = np.linspace(0.0, 1.0, L, dtype=np.float32)
    w_ang = (2.0 * math.pi * np.arange(L, dtype=np.float32) / L).astype(np.float32)
    bands = np.linspace(1e-4, 15.0, 16, dtype=np.float32)
    ang = (w_ang[:, None] * bands[None, :]).astype(np.float32)
    zpos = np.concatenate([t[:, None], np.cos(ang), -np.sin(ang)], axis=-1).astype(np.float32)
    idxA = L - 1 - np.arange(L)
    idxB = np.minimum(1 + np.arange(L), L - 1)
    zA = np.ascontiguousarray(zpos[idxA].T)
    zB = np.ascontiguousarray(zpos[idxB].T)
    deltas = np.abs(np.linspace(math.log(1e-2) / 1.5, math.log(1e-2) / 0.3, 1024, dtype=np.float32))
    J = np.ascontiguousarray(np.eye(128, dtype=np.float32)[::-1])
    blk = np.zeros((128, 128), np.float32)
    blk[:64, :64] = 1.0 / 64
    blk[64:, 64:] = 1.0 / 64
    return zA, zB, deltas, np.ascontiguousarray(np.stack([J, blk], axis=1))


HY_ZA, HY_ZB, HY_DELTAS, CST2 = _hy_consts()


def _fft_consts():
    N = 2 * L
    n1 = np.arange(128); k1 = np.arange(128); n2 = np.arange(256); k2 = np.arange(256)
    cs = lambda th: (np.cos(th), np.sin(th))
    c1, s1 = cs(2 * np.pi * np.outer(np.arange(64), k1) / 128)
    G1 = np.zeros((128, 256))
    G1[0:64, 0:128] = c1; G1[0:64, 128:256] = -s1
    G1[64:128, 0:128] = s1; G1[64:128, 128:256] = c1
    c1f, s1f = cs(2 * np.pi * np.outer(n1, k1) / 128)
    G1f = np.concatenate([c1f, -s1f], axis=1)
    twc, tws = cs(2 * np.pi * np.outer(n2, k1) / N)
    c3, s3 = cs(2 * np.pi * np.outer(n2, k2) / 256)
    blk = lambda m: m.reshape(2, 128, 2, 128).transpose(1, 0, 2, 3).reshape(128, 512)
    C3, S3 = blk(c3), blk(s3)
    c5, s5 = c3.T, s3.T
    G5 = np.zeros((128, 2, 2, 512))
    for kb in range(2):
        rows = slice(128 * kb, 128 * kb + 128)
        G5[:, kb, 0, 0:256] = c5[rows]; G5[:, kb, 0, 256:512] = s5[rows]
        G5[:, kb, 1, 0:256] = -s5[rows]; G5[:, kb, 1, 256:512] = c5[rows]
    c7, s7 = cs(2 * np.pi * np.outer(k1, np.arange(64)) / 128)
    G7 = np.zeros((128, 2, 128))
    G7[:, 0, 0:64] = c7; G7[:, 0, 64:128] = s7
    G7[:, 1, 0:64] = -s7; G7[:, 1, 64:128] = c7
    TC = twc.reshape(2, 128, 128).transpose(1, 0, 2).reshape(128, 256)
    TS = tws.reshape(2, 128, 128).transpose(1, 0, 2).reshape(128, 256)
    TCT, TST = twc.T, tws.T
    SG = np.tile((np.where(k1 % 2 == 0, 1.0, -1.0) / N)[None, :], (128, 1))
    tabs = [G1, G1f, C3, S3, -S3, G5.reshape(128, 2048), G7.reshape(128, 256), TC, TS, TCT, TST, SG]
    return np.ascontiguousarray(np.concatenate(tabs, axis=1).astype(np.float32))


FFTC = _fft_consts()
NFC = FFTC.shape[1]
NHC = 18


def host_prep(inputs):
    f = lambda a: np.ascontiguousarray(np.asarray(a, dtype=np.float32))
    x = f(inputs["x"])
    c = f(inputs["c"])
    ctx = f(inputs["ctx"])
    c_ctx = f(inputs["c_ctx"])
    w_in = f(inputs["w_in"])[0]
    maps = []
    w_out = f(inputs["w_out"])[0]
    wqT = np.ascontiguousarray(f(inputs["peer_wq"])[0].T)
    skT = np.ascontiguousarray(np.transpose(f(inputs["peer_subkeys"])[0], (2, 0, 1)))
    u2 = np.ascontiguousarray(np.transpose(f(inputs["peer_u"])[0].reshape(128, 128, D), (2, 1, 0)))
    v2 = np.ascontiguousarray(f(inputs["peer_v"])[0].reshape(128, 128, D))
    iota = np.ascontiguousarray(np.tile(np.arange(128, dtype=np.float32)[None, :], (128, 1)))
    for k in range(NCORE):
        b = k // 4
        gi = k % 4
        cols = []
        for j in range(3):
            cols.append(np.arange(OFF_HY + j * 1024 + 128 * k, OFF_HY + j * 1024 + 128 * k + 128))
        cols.append(np.arange(256 * gi, 256 * gi + 256))
        cols.append(np.arange(OFF_XBC + 256 * gi, OFF_XBC + 256 * gi + 256))
        cols.append(np.arange(OFF_XBC + 1024 + 128 * gi, OFF_XBC + 1024 + 128 * gi + 128))
        cols.append(np.arange(OFF_XBC + 1536 + 128 * gi, OFF_XBC + 1536 + 128 * gi + 128))
        cols.append(np.arange(OFF_DT + 4 * gi, OFF_DT + 4 * gi + 4))
        cols.append(np.arange(OFF_DT + 16 + 4 * gi, OFF_DT + 16 + 4 * gi + 4))
        cols = np.concatenate(cols)
        colv = np.concatenate([f(inputs["b_mod"])[0][:2048].reshape(16, 128).T, f(inputs["norm1"])[0].reshape(8, 128).T], axis=1)
        cw = f(inputs["ssd_conv_w"])[0]
        cb = f(inputs["ssd_conv_b"])[0]
        xcols = [np.arange(256 * gi, 256 * gi + 128), np.arange(256 * gi + 128, 256 * gi + 256),
                 np.arange(1024 + 128 * gi, 1024 + 128 * gi + 128), np.arange(1536 + 128 * gi, 1536 + 128 * gi + 128)]
        extra = []
        for blk in range(4):
            for tap in range(5):
                extra.append(cw[tap, xcols[blk]])
        for blk in range(4):
            extra.append(cb[xcols[blk]])
        dtb = f(inputs["ssd_dt_bias"])[0]
        alog = f(inputs["ssd_a_log"])[0]
        for dd in range(2):
            for h in range(4):
                extra.append(np.full(128, dtb[dd, 4 * gi + h], np.float32))
        for dd in range(2):
            for h in range(4):
                extra.append(np.full(128, alog[dd, 4 * gi + h], np.float32))
        sd = f(inputs["ssd_d"])[0]
        for h in range(4):
            extra.append(np.full(128, sd[4 * gi + h], np.float32))
        sn = f(inputs["ssd_norm"])[0]
        extra.append(sn[256 * gi:256 * gi + 128])
        extra.append(sn[256 * gi + 128:256 * gi + 256])
        colv = np.ascontiguousarray(np.concatenate([colv, np.stack(extra, axis=1)], axis=1))
        assert colv.shape == (128, NCOLV)
        rowv = np.ascontiguousarray(np.concatenate([cb[xcols[0]], cb[xcols[1]], cb[xcols[2]]])[None, :])
        hsw = f(inputs["hy_short_w"])[0]
        hsb = f(inputs["hy_short_b"])[0]
        hc = []
        for j in range(3):
            cc = np.arange(j * 1024 + 128 * k, j * 1024 + 128 * k + 128)
            for tap in range(3):
                hc.append(hsw[tap, cc])
        for j in range(3):
            hc.append(hsb[j * 1024 + 128 * k: j * 1024 + 128 * k + 128])
        fbias = f(inputs["hy_filt_bias"])[0]
        hc.append(fbias[0, 128 * k:128 * k + 128])
        hc.append(fbias[1, 128 * k:128 * k + 128])
        hc.append(f(inputs["hy_norm"])[0][128 * k:128 * k + 128])
        hc.append(-HY_DELTAS[128 * k:128 * k + 128])
        hc.append(np.full(128, float(b), np.float32))
        hc.append(np.full(128, -math.pi, np.float32))
        hycol = np.ascontiguousarray(np.stack(hc, axis=1))
        assert hycol.shape == (128, NHC)
        hyrow = np.ascontiguousarray(hsb[128 * k:128 * k + 128][None, :])
        pm = np.zeros((NPB,), np.float32)
        for q in range(8):
            hb, tb = q // 4, q % 4
            pm[q] = float(hb == b and tb == gi)
            pm[8 + q] = float(hb == (0 if b == 0 else 1) and tb == gi)
            pm[16 + q] = float(hb == (1 if b == 0 else 0) and tb == gi)
        pbcol = np.ascontiguousarray(np.tile(pm[None, :], (128, 1)))
        w3 = f(inputs["hy_w3"])[0]
        w3k = np.ascontiguousarray(np.stack([w3[:, q * 1024 + 128 * k: q * 1024 + 128 * k + 128] for q in range(4)], axis=1))
        sf = f(inputs["hy_sin_freq"])[0]
        hy64 = np.ascontiguousarray(np.stack([f(inputs["hy_b1"])[0], f(inputs["hy_b2"])[0], sf[0], sf[1]], axis=1))
        m = {
            "x_own": x[b], "x_oth": x[1 - b], "ctx_own": ctx[b],
            "cT": np.ascontiguousarray(np.stack([c[b], c[1 - b], c_ctx], axis=1)),
            "w_mod": f(inputs["w_mod"])[0], "b_mod": f(inputs["b_mod"])[0], "colv": colv,
            "w_k": np.ascontiguousarray(w_in[:, cols]),
            "ident": np.eye(128, dtype=np.float32),
            "rowv": rowv, "cst": CST, "cst2": CST2, "hycol": hycol, "hyrow": hyrow, "w3k": w3k, "hy64": hy64,
            "fftc": FFTC, "hy_w1": f(inputs["hy_w1"])[0], "hy_w2": f(inputs["hy_w2"])[0], "zA": HY_ZA, "zB": HY_ZB,
            "x_res": np.ascontiguousarray(x[b, gi * 4096:(gi + 1) * 4096]), "final_norm": f(inputs["final_norm"]),
            "norm2": f(inputs["norm2"])[0], "w_out": w_out, "wqT": wqT, "skT": skT, "u2": u2, "v2": v2, "pbcol": pbcol, "iota": iota,
        }
        maps.append(m)
    return maps


def kernel(**inputs):
    maps = host_prep(inputs)
    nc = build_nc()
    res = run_bass_kernel_spmd(nc, maps, core_ids=list(range(NCORE)))
    out = np.concatenate([r["out"] for r in res.results], axis=0).reshape(2, L, D)
    return out.astype(np.float32)


def barrier(g):
    S = g.S
    for eng in S.ENG:
        deps = {k: v for k, v in S.cnt.items() if v > 0 and k != eng}
        S._wait(eng, deps)


def bc(ap, axis, shape):
    return ap.unsqueeze(axis).to_broadcast(list(shape))


def phase_ssd(g, dbg):
    S, I, ps, nc, sb, colv = g.S, g.I, g.ps, g.nc, g.sb, g.colv
    nlat = 128 if dbg is None else int(os.environ.get("SSD_NCH", "4"))
    CW = lambda blk, tap: colv.ap[:, 24 + blk * 5 + tap:25 + blk * 5 + tap]
    CB = lambda blk: colv.ap[:, 44 + blk:45 + blk]
    cst = sb("cst", [128, 4, 128], F32)
    S.dma(cst, I.cst)
    DW = sb("DW", [128, 20, 128], BF16)
    for j in range(20):
        S.op("dve", lambda e: e.tensor_scalar(out=DW.ap[:, j, :], in0=g.ident.ap, scalar1=CW(j // 5, j % 5), scalar2=None,
                                              op0=ALU.mult), r=[g.ident, colv], w=[DW])
    DI = sb("DI", [128, 4, 128], BF16)
    for h in range(4):
        S.op("dve", lambda e: e.tensor_scalar(out=DI.ap[:, h, :], in0=g.ident.ap, scalar1=colv.ap[:, 64 + h:65 + h], scalar2=None,
                                              op0=ALU.mult), r=[g.ident, colv], w=[DI])
    ones32 = sb("ones32", [128, 128], F32)
    S.op("dve", lambda e: e.memset(ones32.ap, 1.0), w=[ones32])
    ones256 = sb("ones256", [128, 128], BF16)
    S.op("dve", lambda e: e.memset(ones256.ap, 1.0 / 256), w=[ones256])
    onesrow = sb("onesrow", [1, 128], BF16)
    S.op("dve", lambda e: e.memset(onesrow.ap, 1.0), w=[onesrow])
    browb = sb("browb", [1, 384], BF16)
    S.dma(browb, I.rowv, q="pool")
    onec = sb("onec", [128, 1], F32)
    S.op("dve", lambda e: e.memset(onec.ap, 1.0), w=[onec])
    dt = sb("dt", [128, NCH, 8], F32)
    S.op("dve", lambda e: e.tensor_tensor(out=dt.ap, in0=g.dt_raw.ap, in1=bc(colv.ap[:, 48:56], 1, [128, NCH, 8]), op=ALU.add),
         r=[g.dt_raw, colv], w=[dt])
    S.op("act", lambda e: e.activation(out=dt.ap, in_=dt.ap, func=AF.Exp), r=[dt], w=[dt])
    S.op("act", lambda e: e.activation(out=dt.ap, in_=dt.ap, func=AF.Ln, bias=onec.ap[:, 0:1], scale=1.0), r=[dt, onec], w=[dt])
    lndt = sb("lndt", [128, NCH, 8], F32)
    S.op("act", lambda e: e.activation(out=lndt.ap, in_=dt.ap, func=AF.Ln), r=[dt], w=[lndt])
    Aneg = sb("Aneg", [128, 8], F32)
    S.op("act", lambda e: e.activation(out=Aneg.ap, in_=colv.ap[:, 56:64], func=AF.Exp), r=[colv], w=[Aneg])
    S.op("dve", lambda e: e.tensor_scalar(out=Aneg.ap, in0=Aneg.ap, scalar1=-1.0, scalar2=None, op0=ALU.mult), r=[Aneg], w=[Aneg])
    a_d, cs_d, tot_d, w_d, dtot_d, cb_d = [], [], [], [], [], []
    for d in range(2):
        a = sb("a%d" % d, [128, NCH, 4], F32)
        S.op("dve", lambda e: e.tensor_tensor(out=a.ap, in0=dt.ap[:, :, 4 * d:4 * d + 4],
                                              in1=bc(Aneg.ap[:, 4 * d:4 * d + 4], 1, [128, NCH, 4]), op=ALU.mult),
             r=[dt, Aneg], w=[a])
        cs = sb("cs%d" % d, [128, NCH, 4], F32)
        tot = sb("tot%d" % d, [128, NCH, 4], F32)
        af = a.ap.rearrange("p c h -> p (c h)")
        for half in range(2):
            lo, hi = half * 260, (half + 1) * 260
            S.op("pe", lambda e: e.matmul(ps[0].ap[:, 0:260], lhsT=cst.ap[:, d, :], rhs=af[:, lo:hi], start=True, stop=True),
                 r=[cst, a], w=[ps[0]])
            S.op("dve", lambda e: e.tensor_copy(out=cs.ap.rearrange("p c h -> p (c h)")[:, lo:hi], in_=ps[0].ap[:, 0:260]),
                 r=[ps[0]], w=[cs])
            S.op("pe", lambda e: e.matmul(ps[1].ap[:, 0:260], lhsT=ones32.ap, rhs=af[:, lo:hi], start=True, stop=True),
                 r=[ones32, a], w=[ps[1]])
            S.op("dve", lambda e: e.tensor_copy(out=tot.ap.rearrange("p c h -> p (c h)")[:, lo:hi], in_=ps[1].ap[:, 0:260]),
                 r=[ps[1]], w=[tot])
        w = sb("w%d" % d, [128, NCH, 4], F32)
        S.op("dve", lambda e: e.tensor_tensor(out=w.ap, in0=tot.ap, in1=cs.ap, op=ALU.subtract), r=[tot, cs], w=[w])
        S.op("act", lambda e: e.activation(out=w.ap, in_=w.ap, func=AF.Exp), r=[w], w=[w])
        S.op("dve", lambda e: e.tensor_tensor(out=w.ap, in0=w.ap, in1=dt.ap[:, :, 4 * d:4 * d + 4], op=ALU.mult), r=[w, dt], w=[w])
        dtot = sb("dtot%d" % d, [128, NCH, 4], F32)
        S.op("act", lambda e: e.activation(out=dtot.ap, in_=tot.ap, func=AF.Exp), r=[tot], w=[dtot])
        cb = sb("cb%d" % d, [128, NCH, 4], F32)
        S.op("dve", lambda e: e.tensor_tensor(out=cb.ap, in0=cs.ap, in1=lndt.ap[:, :, 4 * d:4 * d + 4], op=ALU.subtract),
             r=[cs, lndt], w=[cb])
        a_d.append(a); cs_d.append(cs); tot_d.append(tot); w_d.append(w); dtot_d.append(dtot); cb_d.append(cb)
    pin = [sb("pin%d" % i, [128, 4, 132], BF16) for i in range(2)]
    XB = sb("XBtm", [128, 384], BF16)
    BC = sb("BCcm", [128, 2, 128], BF16)
    XP = [sb("XP%d" % h, [128, 128], BF16) for h in range(4)]
    HP = [[sb("HP%d_%d" % (d, h), [128, 128], BF16) for h in range(4)] for d in range(2)]
    for t in XP + HP[0] + HP[1]:
        S.op("pool", lambda e: e.memset(t.ap, 0.0), w=[t])
    H = [sb("H%d" % d, [128, 4, 64], F32) for d in range(2)]
    for d in range(2):
        S.op("pool", lambda e: e.memset(H[d].ap, 0.0), w=[H[d]])
    HBst = sb("HBst", [128, 128, 256], BF16)
    xw = sb("xw", [128, 4, 64], BF16)
    aTri = sb("aTri", [128, 2, 4, 128], F32)
    D1 = sb("D1", [128, 2, 4, 128], F32)
    E = sb("E", [128, 2, 4, 128], BF16)
    M = sb("M", [128, 2, 4, 128], BF16)
    E0 = sb("E0", [128, 2, 4, 128], BF16)
    Cp = sb("Cp", [128, 2, 4, 128], BF16)
    zt = sb("zt", [128, 2, 128], BF16)
    sz = sb("sz", [128, 2, 128], BF16)
    gt = sb("gt", [128, 2, 128], F32)
    sqg = sb("sqg", [128, 2, 128], BF16)
    rs = sb("rs", [128, 128], F32)
    og = sb("og", [128, 2, 128], F32)
    obf = sb("obf", [128, 2, 128], BF16)
    P_CONV, P_CM, P_R0, P_R1, P_G, P_S, P_Y, P_MS = ps[0], ps[1], ps[1], ps[2], ps[3], ps[4], ps[5], ps[6]
    cnt = {"pin": 0}

    def gen(c, need_cm):
        is_ctx = c >= 128
        n = CTX if is_ctx else L
        t0 = (c - 128) * 128 if is_ctx else c * 128
        p = pin[cnt["pin"] % 2]
        cnt["pin"] += 1
        lo, hi = max(t0 - 2, 0), min(t0 + 130, n)
        if is_ctx:
            src = g.PC.v(g.PC.ap[:, :, lo:hi].rearrange("b c t -> c b t"))
        else:
            src = g.PS.v(g.PS.ap[2:6, :, lo:hi].rearrange("b c t -> c b t"))
        if lo != t0 - 2:
            S.op("pool", lambda e: e.memset(p.ap[:, :, 0:2], 0.0), w=[p])
        if hi != t0 + 130:
            S.op("pool", lambda e: e.memset(p.ap[:, :, 130:132], 0.0), w=[p])
        S.dma(p[:, :, lo - (t0 - 2):hi - (t0 - 2)], src, q="sp")
        for blk in range(3):
            o = P_CONV.ap[:, blk * 128:(blk + 1) * 128]
            for tap in range(5):
                S.op("pe", lambda e: e.matmul(o, lhsT=p.ap[:, blk, tap:tap + 128], rhs=DW.ap[:, blk * 5 + tap, :],
                                              start=(tap == 0), stop=False), r=[p, DW], w=[P_CONV], inc=False)
            S.op("pe", lambda e: e.matmul(o, lhsT=onesrow.ap, rhs=browb.ap[0:1, blk * 128:(blk + 1) * 128], start=False, stop=True),
                 r=[onesrow, browb], w=[P_CONV], inc=(blk == 2))
        S.op("act", lambda e: e.activation(out=XB.ap, in_=P_CONV.ap[:, 0:384], func=AF.Silu), r=[P_CONV], w=[XB])
        if need_cm:
            for blk in (2, 3):
                o = P_CM.ap[:, (blk - 2) * 128:(blk - 1) * 128]
                for tap in range(5):
                    S.op("pe", lambda e: e.matmul(o, lhsT=DW.ap[:, blk * 5 + tap, :], rhs=p.ap[:, blk, tap:tap + 128],
                                                  start=(tap == 0), stop=(tap == 4)), r=[p, DW], w=[P_CM], inc=(tap == 4))
                S.op("act", lambda e: e.activation(out=BC.ap[:, blk - 2, :], in_=o, func=AF.Silu, bias=CB(blk), scale=1.0),
                     r=[P_CM, colv], w=[BC])

    def state_update(c, d, store=None):
        S.op("dve", lambda e: e.tensor_tensor(out=xw.ap, in0=XB.ap[:, 0:256].rearrange("p (h q) -> p h q", q=64),
                                              in1=bc(w_d[d].ap[:, c, :], 2, [128, 4, 64]), op=ALU.mult), r=[XB, w_d[d]], w=[xw])
        S.op("pe", lambda e: e.matmul(P_S.ap[:, 0:256], lhsT=XB.ap[:, 256:384], rhs=xw.ap.rearrange("p h q -> p (h q)"),
                                      start=True, stop=True), r=[XB, xw], w=[P_S])
        if store is not None:
            S.op("pool", lambda e: e.tensor_copy(out=HBst.ap[:, store, :], in_=H[d].ap.rearrange("p h q -> p (h q)")),
                 r=[H[d]], w=[HBst])
        S.op("dve", lambda e: e.tensor_tensor(out=H[d].ap, in0=H[d].ap, in1=bc(dtot_d[d].ap[:, c, :], 2, [128, 4, 64]), op=ALU.mult),
             r=[H[d], dtot_d[d]], w=[H[d]])
        S.op("dve", lambda e: e.tensor_tensor(out=H[d].ap, in0=H[d].ap, in1=P_S.ap[:, 0:256].rearrange("p (h q) -> p h q", q=64),
                                              op=ALU.add), r=[H[d], P_S], w=[H[d]])

    def refresh_hp(d, src_ap, src_t):
        for h in range(4):
            off = (h % 2) * 64
            S.op("pool", lambda e: e.tensor_copy(out=HP[d][h].ap[:, off:off + 64], in_=src_ap[:, h * 64:(h + 1) * 64]),
                 r=[src_t], w=[HP[d][h]])

    for c in (128, 129):
        gen(c, False)
        state_update(c, 0)
    for c in (129, 128):
        gen(c, False)
        state_update(c, 1)
    for c in range(nlat - 1, -1, -1):
        gen(c, False)
        state_update(c, 1, store=c)
    refresh_hp(0, H[0].ap.rearrange("p h q -> p (h q)"), H[0])
    for c in range(nlat):
        t0 = c * 128
        gen(c, True)
        S.dma(zt, g.PS.v(g.PS.ap[0:2, :, t0:t0 + 128].rearrange("b c t -> c b t")), q="pool")
        for h in range(4):
            off = (h % 2) * 64
            S.op("pool", lambda e: e.tensor_copy(out=XP[h].ap[:, off:off + 64], in_=XB.ap[:, h * 64:(h + 1) * 64]), r=[XB], w=[XP[h]])
        refresh_hp(1, HBst.ap[:, c, :], HBst)
        S.op("pe", lambda e: e.matmul(P_G.ap[:, 0:128], lhsT=BC.ap[:, 0, :], rhs=BC.ap[:, 1, :], start=True, stop=True),
             r=[BC], w=[P_G])
        PR = (P_R0, P_R1)
        for d in range(2):
            S.op("dve", lambda e: e.tensor_tensor(out=aTri.ap[:, d], in0=bc(cst.ap[:, d, :], 1, [128, 4, 128]),
                                                  in1=bc(a_d[d].ap[:, c, :], 2, [128, 4, 128]), op=ALU.mult),
                 r=[cst, a_d[d]], w=[aTri])
        for d in range(2):
            S.op("pe", lambda e: e.matmul(PR[d].ap, lhsT=ones32.ap, rhs=aTri.ap[:, d].rearrange("p h i -> p (h i)"),
                                          start=True, stop=True), r=[ones32, aTri], w=[PR[d]])
        for d in range(2):
            r3 = PR[d].ap.rearrange("p (h i) -> p h i", i=128)
            S.op("dve", lambda e: e.tensor_tensor(out=D1.ap[:, d], in0=r3, in1=bc(cb_d[d].ap[:, c, :], 2, [128, 4, 128]),
                                                  op=ALU.subtract), r=[PR[d], cb_d[d]], w=[D1])
            S.op("act", lambda e: e.activation(out=E0.ap[:, d], in_=r3, func=AF.Exp), r=[PR[d]], w=[E0])
        for d in range(2):
            S.op("pool", lambda e: e.tensor_tensor(out=D1.ap[:, d], in0=D1.ap[:, d], in1=bc(cst.ap[:, 2 + d, :], 1, [128, 4, 128]),
                                                   op=ALU.add), r=[D1, cst], w=[D1])
        S.op("act", lambda e: e.activation(out=E.ap, in_=D1.ap, func=AF.Exp), r=[D1], w=[E])
        for d in range(2):
            S.op("dve", lambda e: e.tensor_tensor(out=M.ap[:, d], in0=E.ap[:, d], in1=bc(P_G.ap[:, 0:128], 1, [128, 4, 128]),
                                                  op=ALU.mult), r=[E, P_G], w=[M])
            S.op("pool", lambda e: e.tensor_tensor(out=Cp.ap[:, d], in0=E0.ap[:, d], in1=bc(BC.ap[:, 1, :], 1, [128, 4, 128]),
                                                   op=ALU.mult), r=[E0, BC], w=[Cp])
        for hb in range(2):
            o = P_Y.ap[:, hb * 128:(hb + 1) * 128]
            mms = []
            for h in (2 * hb, 2 * hb + 1):
                for d in range(2):
                    mms.append((XP[h], XP[h].ap, M, M.ap[:, d, h, :]))
                mms.append((XP[h], XP[h].ap, DI, DI.ap[:, h, :]))
                for d in range(2):
                    mms.append((HP[d][h], HP[d][h].ap, Cp, Cp.ap[:, d, h, :]))
            for n_, (lt, lap, rt, rap) in enumerate(mms):
                S.op("pe", lambda e: e.matmul(o, lhsT=lap, rhs=rap, start=(n_ == 0), stop=(n_ == len(mms) - 1)),
                     r=[lt, rt], w=[P_Y], inc=(n_ == len(mms) - 1))
        state_update(c, 0)
        refresh_hp(0, H[0].ap.rearrange("p h q -> p (h q)"), H[0])
        S.op("act", lambda e: e.activation(out=sz.ap, in_=zt.ap, func=AF.Silu), r=[zt], w=[sz])
        S.op("dve", lambda e: e.tensor_tensor(out=gt.ap, in0=P_Y.ap[:, 0:256].rearrange("p (b i) -> p b i", i=128), in1=sz.ap,
                                              op=ALU.mult), r=[P_Y, sz], w=[gt])
        S.op("act", lambda e: e.activation(out=sqg.ap, in_=gt.ap, func=AF.Square), r=[gt], w=[sqg])
        for hb in range(2):
            S.op("pe", lambda e: e.matmul(P_MS.ap[:, 0:128], lhsT=ones256.ap, rhs=sqg.ap[:, hb, :], start=(hb == 0), stop=(hb == 1)),
                 r=[ones256, sqg], w=[P_MS], inc=(hb == 1))
        S.op("act", lambda e: e.activation(out=rs.ap, in_=P_MS.ap[:, 0:128], func=AF.Ln, bias=g.epsb.ap[:, 0:1], scale=1.0),
             r=[P_MS, g.epsb], w=[rs])
        S.op("act", lambda e: e.activation(out=rs.ap, in_=rs.ap, func=AF.Exp, scale=-0.5), r=[rs], w=[rs])
        S.op("dve", lambda e: e.tensor_tensor(out=og.ap, in0=gt.ap, in1=bc(rs.ap, 1, [128, 2, 128]), op=ALU.mult), r=[gt, rs], w=[og])
        for hb in range(2):
            S.op("dve", lambda e: e.tensor_scalar(out=obf.ap[:, hb, :], in0=og.ap[:, hb, :], scalar1=colv.ap[:, 68 + hb:69 + hb],
                                                  scalar2=None, op0=ALU.mult), r=[og, colv], w=[obf])
        S.dma(g.OSSD.v(g.OSSD.ap[:, :, t0:t0 + 128].rearrange("b c t -> c b t")), obf, q="pool")


def phase_tail_incomplete(g):
    S, I, sb, nc = g.S, g.I, g.sb, g.nc
    out = T(nc.dram_tensor("out", [4096, D], F32, kind="ExternalOutput").ap(), "out")
    fn = sb("fnbc", [128, D], F32)
    S.dma(fn, I.final_norm.v(I.final_norm.ap.partition_broadcast(128)))
    xt = [sb("xt%d" % i, [128, D], F32) for i in range(2)]
    junk = sb("junk", [128, D], BF16)
    ss = [sb("ss%d" % i, [128, 1], F32) for i in range(2)]
    ot = [sb("ot%d" % i, [128, D], F32) for i in range(2)]
    for i in range(32):
        a = i % 2
        S.dma(xt[a], I.x_res[i * 128:(i + 1) * 128, :], q="sp")
        S.op("act", lambda e: e.activation(out=junk.ap, in_=xt[a].ap, func=AF.Square, accum_out=ss[a].ap), r=[xt[a]], w=[junk, ss[a]])
        S.op("act", lambda e: e.activation(out=ss[a].ap, in_=ss[a].ap, func=AF.Ln, bias=g.epsb.ap[:, 0:1], scale=1.0 / D),
             r=[ss[a], g.epsb], w=[ss[a]])
        S.op("act", lambda e: e.activation(out=ss[a].ap, in_=ss[a].ap, func=AF.Exp, scale=-0.5), r=[ss[a]], w=[ss[a]])
        S.op("dve", lambda e: e.scalar_tensor_tensor(out=ot[a].ap, in0=xt[a].ap, scalar=ss[a].ap[:, 0:1], in1=fn.ap,
                                                     op0=ALU.mult, op1=ALU.mult), r=[xt[a], ss[a], fn], w=[ot[a]])
        S.dma(out[i * 128:(i + 1) * 128, :], ot[a], q="pool")
    S.finish([out])


KXW = 2 * L + 128
TWO_PI = 2.0 * math.pi


def phase_hyena(g, dbg):
    S, I, ps, nc, sb, st0 = g.S, g.I, g.ps, g.nc, g.sb, g.cur
    g.KXR = [g.dram_tmp("KXR%d" % o, [128, KXW], BF16) for o in range(2)]
    g.X1 = g.dram_tmp("X1", [128, 2, L], BF16)
    g.X2P = g.dram_tmp("X2P", [128, 2, L], BF16)
    g.Z1P = g.dram_tmp("Z1P", [128, 2, L], BF16)
    hycol = sb("hycol", [128, NHC], F32)
    S.dma(hycol, I.hycol)
    c2 = sb("cst2f", [128, 2, 128], F32)
    S.dma(c2, I.cst2)
    Jb = sb("Jb", [128, 128], BF16)
    S.op("dve", lambda e: e.tensor_copy(out=Jb.ap, in_=c2.ap[:, 0, :]), r=[c2], w=[Jb])
    blk64 = sb("blk64", [128, 128], BF16)
    S.op("dve", lambda e: e.tensor_copy(out=blk64.ap, in_=c2.ap[:, 1, :]), r=[c2], w=[blk64])
    DW3 = sb("DW3", [128, 9, 128], BF16)
    for j in range(9):
        S.op("dve", lambda e: e.tensor_scalar(out=DW3.ap[:, j, :], in0=g.ident.ap, scalar1=hycol.ap[:, j:j + 1], scalar2=None,
                                              op0=ALU.mult), r=[g.ident, hycol], w=[DW3])
    onesrow = sb("onesrow_h", [1, 128], BF16)
    S.op("dve", lambda e: e.memset(onesrow.ap, 1.0), w=[onesrow])
    hyrowb = sb("hyrowb", [1, 128], BF16)
    S.dma(hyrowb, I.hyrow, q="pool")
    UT = sb("UT", [128, 128, 256], BF16)
    UTc = [T(UT.ap[:, c, :], "UT%d" % c) for c in range(128)]
    use_fft = os.environ.get("HY_TOEPLITZ") is None

    def ut_col(j, s_):
        return (j % 2) * 128 + s_ * 64 + j // 2 if use_fft else 2 * j + s_

    def ut_blk4(j0, s_):
        if use_fft:
            base = s_ * 64 + j0 // 2
            v = UT.ap.rearrange("p c (l x) -> p c l x", l=2)[:, :, :, base:base + 2]
            return v.rearrange("p c l h -> p h l c"), (lambda a: a.rearrange("p (h l c) -> p h l c", h=2, l=2))
        jb0 = j0 * 2 + s_
        return UT.ap[:, :, jb0:jb0 + 7:2].rearrange("p c j -> p j c"), (lambda a: a.rearrange("p (j c) -> p j c", c=128))
    nblk = 32 if dbg is None else int(os.environ.get("HY_NBLK", "32"))

    with ExitStack() as sc:
        g.cur = sc
        w1 = sb("hw1", [33, 64], F32)
        S.dma(w1, I.hy_w1)
        w2 = sb("hw2", [64, 64], F32)
        S.dma(w2, I.hy_w2)
        w3k = sb("hw3k", [64, 4, 128], F32)
        S.dma(w3k, I.w3k)
        h64 = sb("h64", [64, 4], F32)
        S.dma(h64, I.hy64)
        fb = sb("fb", [64, 2], F32)
        S.op("dve", lambda e: e.tensor_tensor(out=fb.ap, in0=h64.ap[:, 0:2], in1=h64.ap[:, 2:4], op=ALU.mult), r=[h64], w=[fb])
        zt = [sb("zt%d" % i, [33, 512], F32) for i in range(2)]
        tp = [sb("tp%d" % i, [128, 512], F32) for i in range(2)]
        u = [sb("hu%d" % i, [64, 512], F32) for i in range(2)]
        m = sb("hm", [64, 512], F32)
        hh = [sb("hh%d" % i, [64, 512], F32) for i in range(2)]
        dec = sb("dec", [128, 512], F32)
        kf = [sb("kf%d" % i, [128, 512], F32) for i in range(2)]
        kb = [sb("kb%d" % i, [128, 512], BF16) for i in range(4)]
        n = 0

        def sin_layer(P, layer, ui, out):
            S.op("dve", lambda e: e.tensor_scalar(out=ui.ap, in0=P.ap[0:64, :], scalar1=h64.ap[:, 2 + layer:3 + layer],
                                                  scalar2=fb.ap[:, layer:layer + 1], op0=ALU.mult, op1=ALU.add),
                 r=[P, h64, fb], w=[ui])
            for rep in range(2):
                S.op("dve", lambda e: e.tensor_scalar(out=m.ap, in0=ui.ap, scalar1=math.pi, scalar2=-TWO_PI, op0=ALU.is_gt,
                                                      op1=ALU.mult), r=[ui], w=[m])
                S.op("dve", lambda e: e.tensor_tensor(out=ui.ap, in0=ui.ap, in1=m.ap, op=ALU.add), r=[ui, m], w=[ui])
                S.op("dve", lambda e: e.tensor_scalar(out=m.ap, in0=ui.ap, scalar1=-math.pi, scalar2=TWO_PI, op0=ALU.is_lt,
                                                      op1=ALU.mult), r=[ui], w=[m])
                S.op("dve", lambda e: e.tensor_tensor(out=ui.ap, in0=ui.ap, in1=m.ap, op=ALU.add), r=[ui, m], w=[ui])
            S.op("act", lambda e: e.activation(out=out.ap, in_=ui.ap, func=AF.Sin), r=[ui], w=[out])

        for tbl, (zsrc, base) in enumerate(((I.zA, 1), (I.zB, L + 1))):
            for blk in range(L // 512):
                i = n % 2
                n += 1
                cs_ = slice(blk * 512, (blk + 1) * 512)
                S.dma(zt[i], zsrc[:, cs_], q="sp")
                S.dma(tp[i], zsrc.v(zsrc.ap[0, cs_].partition_broadcast(128)), q="pool")
                S.op("pe", lambda e: e.matmul(ps[0].ap[0:64, :], lhsT=w1.ap, rhs=zt[i].ap, start=True, stop=True), r=[w1, zt[i]], w=[ps[0]])
                sin_layer(ps[0], 0, u[0], hh[0])
                S.op("pe", lambda e: e.matmul(ps[1].ap[0:64, :], lhsT=w2.ap, rhs=hh[0].ap, start=True, stop=True), r=[w2, hh[0]], w=[ps[1]])
                sin_layer(ps[1], 1, u[1], hh[1])
                S.op("act", lambda e: e.activation(out=dec.ap, in_=tp[i].ap, func=AF.Exp, scale=hycol.ap[:, 15:16]),
                     r=[tp[i], hycol], w=[dec])
                for o in range(2):
                    q = o * 2 + tbl
                    P3 = ps[2 + o]
                    S.op("pe", lambda e: e.matmul(P3.ap, lhsT=w3k.ap[:, q, :], rhs=hh[1].ap, start=True, stop=True), r=[w3k, hh[1]], w=[P3])
                    S.op("dve", lambda e: e.tensor_tensor(out=kf[o].ap, in0=P3.ap, in1=dec.ap, op=ALU.mult), r=[P3, dec], w=[kf[o]])
                    if tbl == 0 and blk == L // 512 - 1:
                        S.op("dve", lambda e: e.tensor_scalar(out=kf[o].ap[:, 511:512], in0=kf[o].ap[:, 511:512],
                                                              scalar1=hycol.ap[:, 12 + o:13 + o], scalar2=None, op0=ALU.add),
                             r=[kf[o], hycol], w=[kf[o]])
                    kbt = kb[(2 * n + o) % 4]
                    S.op("act", lambda e: e.copy(out=kbt.ap, in_=kf[o].ap), r=[kf[o]], w=[kbt])
                    S.dma(g.KXR[o][:, base + blk * 512: base + (blk + 1) * 512], kbt, q="pool")
        barrier(g)
    g.cur = st0

    if use_fft:
        tb = sb("fft_tb", [128, 4352], BF16)
        tf = sb("fft_tf", [128, 1152], F32)
        S.dma(tb, I.fftc[:, 0:4352], q="pool")
        S.dma(tf, I.fftc[:, 4352:5504], q="sp")
        G1 = tb.ap[:, 0:256]
        G1f = tb.ap[:, 256:512]
        C3 = lambda h, kb: tb.ap[:, 512 + (h * 2 + kb) * 128: 512 + (h * 2 + kb + 1) * 128]
        S3 = lambda h, kb: tb.ap[:, 1024 + (h * 2 + kb) * 128: 1024 + (h * 2 + kb + 1) * 128]
        NS3 = lambda h, kb: tb.ap[:, 1536 + (h * 2 + kb) * 128: 1536 + (h * 2 + kb + 1) * 128]
        G5 = lambda kb, ri: tb.ap[:, 2048 + (kb * 2 + ri) * 512: 2048 + (kb * 2 + ri + 1) * 512]
        G7 = lambda ri: tb.ap[:, 4096 + ri * 128: 4096 + (ri + 1) * 128]
        TC = tf.ap[:, 0:256].rearrange("p (h k) -> p h k", k=128)
        TS = tf.ap[:, 256:512].rearrange("p (h k) -> p h k", k=128)
        TCT = tf.ap[:, 512:768]
        TST = tf.ap[:, 768:1024]
        SG = tf.ap[:, 1024:1152]
        g.KF = [g.dram_tmp("KF%d" % o, [128, 2, 128, 2, 128], F32) for o in range(2)]

    def fwd_group(lhs_of, lhs_t, Gmat, Bt, tt):
        for ci in range(4):
            bank = ps[ci]
            for h in range(2):
                S.op("pe", lambda e: e.matmul(bank.ap[:, h * 256:(h + 1) * 256], lhsT=lhs_of(ci, h), rhs=Gmat, start=True, stop=True),
                     r=[lhs_t, tb], w=[bank], inc=(h == 1))
            A4 = bank.ap.rearrange("p (h r k) -> p h r k", h=2, r=2)
            t1, t2 = tt[ci % 2]
            t14 = t1.ap.rearrange("p (h r k) -> p h r k", h=2, r=2)
            t24 = t2.ap.rearrange("p (h r k) -> p h r k", h=2, r=2)
            S.op("dve", lambda e: e.tensor_tensor(out=t14, in0=A4, in1=TC.unsqueeze(2).to_broadcast([128, 2, 2, 128]), op=ALU.mult),
                 r=[bank, tf], w=[t1])
            S.op("dve", lambda e: e.tensor_tensor(out=t24, in0=A4, in1=TS.unsqueeze(2).to_broadcast([128, 2, 2, 128]), op=ALU.mult),
                 r=[bank, tf], w=[t2])
            S.op("pool", lambda e: e.tensor_tensor(out=Bt.ap[:, :, ci, 0, :], in0=t14[:, :, 0, :], in1=t24[:, :, 1, :], op=ALU.add),
                 r=[t1, t2], w=[Bt])
            S.op("pool", lambda e: e.tensor_tensor(out=Bt.ap[:, :, ci, 1, :], in0=t14[:, :, 1, :], in1=t24[:, :, 0, :], op=ALU.subtract),
                 r=[t1, t2], w=[Bt])
        for kb in range(2):
            Xre, Xim = ps[4 + 2 * kb], ps[5 + 2 * kb]
            mm_re = [(C3(0, kb), 0, 0), (S3(0, kb), 0, 1), (C3(1, kb), 1, 0), (S3(1, kb), 1, 1)]
            mm_im = [(C3(0, kb), 0, 1), (NS3(0, kb), 0, 0), (C3(1, kb), 1, 1), (NS3(1, kb), 1, 0)]
            for bank, mms in ((Xre, mm_re), (Xim, mm_im)):
                for n_, (lt, h, ri) in enumerate(mms):
                    S.op("pe", lambda e: e.matmul(bank.ap, lhsT=lt, rhs=Bt.ap[:, h, :, ri, :], start=(n_ == 0), stop=(n_ == 3)),
                         r=[tb, Bt], w=[bank], inc=(n_ == 3))

    def fft_prepare_filters():
        with ExitStack() as sc2:
            g.cur = sc2
            Bt = sb("fBt", [128, 2, 4, 2, 128], BF16)
            tt = [(sb("ft1_%d" % i, [128, 512], F32), sb("ft2_%d" % i, [128, 512], F32)) for i in range(2)]
            Ft = [sb("fFt%d" % i, [128, 4, 256], BF16) for i in range(2)]
            KFt = [sb("fKFt%d" % i, [128, 2, 4, 2, 128], F32) for i in range(2)]
            for o in range(2):
                for grp in range(32):
                    c0 = 4 * grp
                    f_ = Ft[grp % 2]
                    S.dma(f_, g.KXR[o].v(g.KXR[o].ap[c0:c0 + 4, 0:2 * L].rearrange("c (n m) -> n c m", m=256)), q="sp")
                    S.op("dve", lambda e: e.memset(f_.ap[0:1, :, 0:1], 0.0), w=[f_])
                    fwd_group(lambda ci, h: f_.ap[:, ci, h * 128:(h + 1) * 128], f_, G1f, Bt, tt)
                    kf = KFt[grp % 2]
                    for kb in range(2):
                        xr = ps[4 + 2 * kb].ap.rearrange("p (c k) -> p c k", k=128)
                        xi = ps[5 + 2 * kb].ap.rearrange("p (c k) -> p c k", k=128)
                        sgb = SG.unsqueeze(1).to_broadcast([128, 4, 128])
                        S.op("dve", lambda e: e.tensor_tensor(out=kf.ap[:, kb, :, 0, :], in0=xr, in1=sgb, op=ALU.mult),
                             r=[ps[4 + 2 * kb], tf], w=[kf])
                        S.op("dve", lambda e: e.scalar_tensor_tensor(out=kf.ap[:, kb, :, 1, :], in0=xi, scalar=-1.0, in1=sgb,
                                                                     op0=ALU.mult, op1=ALU.mult), r=[ps[5 + 2 * kb], tf], w=[kf])
                    S.dma(g.KF[o][:, :, c0:c0 + 4, :, :], kf, q="pool")
            barrier(g)
        g.cur = st0

    def fft_conv(o):
        with ExitStack() as sc2:
            g.cur = sc2
            S0 = sb("S0_%d" % o, [128, 128, 256], BF16)
            S0g = [T(S0.ap[:, 4 * i:4 * i + 4, :], "S0g%d" % i) for i in range(32)]
            Bt = sb("cBt_%d" % o, [128, 2, 4, 2, 128], BF16)
            tt = [(sb("ct1_%d_%d" % (o, i), [128, 512], F32), sb("ct2_%d_%d" % (o, i), [128, 512], F32)) for i in range(2)]
            KFt = sb("cKFt_%d" % o, [128, 2, 4, 2, 128], F32)
            mq = [sb("cm%d_%d" % (o, i), [128, 4, 128], F32) for i in range(4)]
            Y = sb("cY_%d" % o, [128, 2, 4, 2, 128], BF16)
            Dt = sb("cDt_%d" % o, [128, 4, 2, 256], BF16)
            for c0 in range(0, 128, 2):
                bank = ps[(c0 // 2) % 4]
                for q in range(4):
                    c, jj = c0 + q // 2, q % 2
                    src = UT.ap[:, c, jj * 128:(jj + 1) * 128]
                    S.op("pe", lambda e: e.matmul(bank.ap[:, q * 128:(q + 1) * 128], lhsT=src, rhs=g.identb.ap, start=True, stop=True),
                         r=[UTc[c], g.identb], w=[bank], inc=(q == 3))
                S.op(("act" if (c0 // 2) % 2 == 0 else "dve"),
                     (lambda e: e.copy(out=S0.ap[:, c0:c0 + 2, :], in_=bank.ap.rearrange("p (c m) -> p c m", m=256))) if (c0 // 2) % 2 == 0 else
                     (lambda e: e.tensor_copy(out=S0.ap[:, c0:c0 + 2, :], in_=bank.ap.rearrange("p (c m) -> p c m", m=256))),
                     r=[bank], w=[S0g[c0 // 4]])
            for grp in range(32):
                c0 = 4 * grp
                sg_ = S0g[grp]
                S.dma(KFt, g.KF[o][:, :, c0:c0 + 4, :, :], q="sp")
                fwd_group(lambda ci, h: S0.ap[:, c0 + ci, h * 128:(h + 1) * 128], sg_, G1, Bt, tt)
                for kb in range(2):
                    xr = ps[4 + 2 * kb].ap.rearrange("p (c k) -> p c k", k=128)
                    xi = ps[5 + 2 * kb].ap.rearrange("p (c k) -> p c k", k=128)
                    kr, ki = KFt.ap[:, kb, :, 0, :], KFt.ap[:, kb, :, 1, :]
                    for mt, xin, kin, bk in ((mq[0], xr, kr, 4 + 2 * kb), (mq[1], xi, ki, 5 + 2 * kb), (mq[2], xr, ki, 4 + 2 * kb), (mq[3], xi, kr, 5 + 2 * kb)):
                        S.op("dve", lambda e: e.tensor_tensor(out=mt.ap, in0=xin, in1=kin, op=ALU.mult), r=[ps[bk], KFt], w=[mt])
                    S.op("pool", lambda e: e.tensor_tensor(out=Y.ap[:, kb, :, 0, :], in0=mq[0].ap, in1=mq[1].ap, op=ALU.subtract),
                         r=[mq[0], mq[1]], w=[Y])
                    S.op("pool", lambda e: e.tensor_tensor(out=Y.ap[:, kb, :, 1, :], in0=mq[2].ap, in1=mq[3].ap, op=ALU.add),
                         r=[mq[2], mq[3]], w=[Y])
                for ci in range(4):
                    bank = ps[ci]
                    n_ = 0
                    for kb in range(2):
                        for ri in range(2):
                            S.op("pe", lambda e: e.matmul(bank.ap, lhsT=Y.ap[:, kb, ci, ri, :], rhs=G5(kb, ri), start=(n_ == 0), stop=(n_ == 3)),
                                 r=[Y, tb], w=[bank], inc=(n_ == 3))
                            n_ += 1
                    D3 = bank.ap.rearrange("p (r m) -> p r m", r=2)
                    t1, t2 = tt[ci % 2]
                    t13 = t1.ap.rearrange("p (r m) -> p r m", r=2)
                    t23 = t2.ap.rearrange("p (r m) -> p r m", r=2)
                    S.op("dve", lambda e: e.tensor_tensor(out=t13, in0=D3, in1=TCT.unsqueeze(1).to_broadcast([128, 2, 256]), op=ALU.mult),
                         r=[bank, tf], w=[t1])
                    S.op("dve", lambda e: e.tensor_tensor(out=t23, in0=D3, in1=TST.unsqueeze(1).to_broadcast([128, 2, 256]), op=ALU.mult),
                         r=[bank, tf], w=[t2])
                    S.op("pool", lambda e: e.tensor_tensor(out=Dt.ap[:, ci, 0, :], in0=t13[:, 0, :], in1=t23[:, 1, :], op=ALU.subtract),
                         r=[t1, t2], w=[Dt])
                    S.op("pool", lambda e: e.tensor_tensor(out=Dt.ap[:, ci, 1, :], in0=t23[:, 0, :], in1=t13[:, 1, :], op=ALU.add),
                         r=[t1, t2], w=[Dt])
                for half in range(2):
                    bank = ps[4 + half]
                    for ri in range(2):
                        S.op("pe", lambda e: e.matmul(bank.ap, lhsT=G7(ri), rhs=Dt.ap[:, 2 * half:2 * half + 2, ri, :], start=(ri == 0), stop=(ri == 1)),
                             r=[tb, Dt], w=[bank], inc=(ri == 1))
                    S.op("act", lambda e: e.copy(out=S0.ap[:, c0 + 2 * half:c0 + 2 * half + 2, :], in_=bank.ap.rearrange("p (c m) -> p c m", m=256)),
                         r=[bank], w=[sg_])
            for c0 in range(0, 128, 2):
                bank = ps[(c0 // 2) % 4]
                for q in range(4):
                    c, jj = c0 + q // 2, q % 2
                    S.op("pe", lambda e: e.matmul(bank.ap[:, q * 128:(q + 1) * 128], lhsT=S0.ap[:, c, jj * 128:(jj + 1) * 128], rhs=g.identb.ap,
                                                  start=True, stop=True), r=[S0g[c // 4], g.identb], w=[bank], inc=(q == 3))
                dst = UT.ap[:, c0:c0 + 2, :]
                src = bank.ap.rearrange("p (c m) -> p c m", m=256)
                if (c0 // 2) % 2 == 0:
                    S.op("act", lambda e: e.copy(out=dst, in_=src), r=[bank], w=[UTc[c0], UTc[c0 + 1]])
                else:
                    S.op("dve", lambda e: e.tensor_copy(out=dst, in_=src), r=[bank], w=[UTc[c0], UTc[c0 + 1]])
            barrier(g)
        g.cur = st0

    if use_fft:
        fft_prepare_filters()
    long_conv = fft_conv if use_fft else None
    Jmat = g.identb if use_fft else Jb

    def perm_copy(eng, dst, src, to_colmajor):
        if to_colmajor:
            o_ = dst.ap.rearrange("p (w r) -> p w r", r=256)
            i_ = src.ap.rearrange("p (r w) -> p w r", w=64)
        else:
            o_ = dst.ap.rearrange("p (r w) -> p r w", w=64)
            i_ = src.ap.rearrange("p (w r) -> p r w", r=256)
        for hf in range(2):
            if to_colmajor:
                S.op(eng, lambda e: e.tensor_copy(out=o_[:, hf * 32:(hf + 1) * 32, :], in_=i_[:, hf * 32:(hf + 1) * 32, :]), r=[src], w=[dst])
            else:
                S.op(eng, lambda e: e.tensor_copy(out=o_[:, hf * 128:(hf + 1) * 128, :], in_=i_[:, hf * 128:(hf + 1) * 128, :]), r=[src], w=[dst])

    def toeplitz_conv(o):
        with ExitStack() as sc2:
            g.cur = sc2
            toe = [sb("toe%d_%d" % (o, i), [128, 8192], BF16) for i in range(4)]
            kx = g.KXR[o]
            for c in range(128 if dbg is None else int(os.environ.get("HY_NCH", "128"))):
                P = ps[c % 2]
                first = True
                order = (1, 0, 2, 3)
                for qi in order:
                    tq = toe[qi]
                    src = bass.AP(kx.ap.tensor, c * KXW + 1 + 8192 * qi, [[1, 128], [1, 8192]])
                    S.dma(tq, kx.v(src), q=("sp" if qi % 2 == 0 else "pool"))
                for qi in order:
                    tq = toe[qi]
                    ms_ = list(range(63, -1, -1)) if qi == 1 else list(range(64))
                    for mm in ms_:
                        d = 127 - (64 * qi + mm)
                        if d < -127:
                            continue
                        j0, j1 = max(0, -d), min(127, 127 - d)
                        last = (qi == order[-1] and mm == 62)
                        S.op("pe", lambda e: e.matmul(P.ap[:, 2 * (j0 + d):2 * (j1 + d + 1)], lhsT=tq.ap[:, mm * 128:(mm + 1) * 128],
                                                      rhs=UTc[c].ap[:, 2 * j0:2 * (j1 + 1)], start=first, stop=last),
                             r=[tq, UTc[c]], w=[P], inc=last)
                        first = False
                if c % 2 == 0:
                    S.op("act", lambda e: e.copy(out=UTc[c].ap, in_=P.ap[:, 0:256]), r=[P], w=[UTc[c]])
                else:
                    S.op("dve", lambda e: e.tensor_copy(out=UTc[c].ap, in_=P.ap[:, 0:256]), r=[P], w=[UTc[c]])
            barrier(g)
        g.cur = st0

    with ExitStack() as sc:
        g.cur = sc
        pin3 = [sb("pin3_%d" % i, [128, 3, 514], BF16) for i in range(2)]
        xo = [sb("xo%d" % i, [128, 512], BF16) for i in range(2)]
        x2n = sb("x2n", [128, L], BF16)
        x2p = sb("x2p", [128, L], BF16)
        vt = [sb("vt%d" % i, [128, 512], BF16) for i in range(2)]
        n = 0
        for b in range(2):
            for bi in range(nblk):
                t0 = bi * 512
                p = pin3[n % 2]
                lo, hi = max(t0 - 1, 0), min(t0 + 513, L)
                if lo != t0 - 1:
                    S.op("pool", lambda e: e.memset(p.ap[:, :, 0:1], 0.0), w=[p])
                if hi != t0 + 513:
                    S.op("pool", lambda e: e.memset(p.ap[:, :, 513:514], 0.0), w=[p])
                S.dma(p[:, :, lo - (t0 - 1):hi - (t0 - 1)], g.PH.v(g.PH.ap[:, :, b, lo:hi].rearrange("j c t -> c j t")), q="sp")
                Pv = ps[2]
                for sbk in range(4):
                    o_ = Pv.ap[:, sbk * 128:(sbk + 1) * 128]
                    for tap in range(3):
                        S.op("pe", lambda e: e.matmul(o_, lhsT=p.ap[:, 0, sbk * 128 + tap: sbk * 128 + tap + 128], rhs=DW3.ap[:, tap, :],
                                                      start=(tap == 0), stop=False), r=[p, DW3], w=[Pv], inc=False)
                    S.op("pe", lambda e: e.matmul(o_, lhsT=onesrow.ap, rhs=hyrowb.ap, start=False, stop=True),
                         r=[onesrow, hyrowb], w=[Pv], inc=(sbk == 3))
                uo, ur = ut_blk4(bi * 4, b)
                S.op("act", lambda e: e.copy(out=uo, in_=ur(Pv.ap)), r=[Pv], w=UTc)
                for ti in (1, 2):
                    Px = ps[2 + ti]
                    for tap in range(3):
                        S.op("pe", lambda e: e.matmul(Px.ap, lhsT=DW3.ap[:, ti * 3 + tap, :], rhs=p.ap[:, ti, tap:tap + 512],
                                                      start=(tap == 0), stop=(tap == 2)), r=[p, DW3], w=[Px], inc=(tap == 2))
                    if ti == 1:
                        xt_ = xo[n % 2]
                        S.op("act", lambda e: e.activation(out=xt_.ap, in_=Px.ap, func=AF.Identity, bias=hycol.ap[:, 10:11], scale=1.0),
                             r=[Px, hycol], w=[xt_])
                        S.dma(g.X1[:, b, t0:t0 + 512], xt_, q="pool")
                    else:
                        S.op("act", lambda e: e.activation(out=x2n.ap[:, t0:t0 + 512], in_=Px.ap, func=AF.Identity,
                                                           bias=hycol.ap[:, 11:12], scale=1.0), r=[Px, hycol], w=[x2n])
                n += 1
            if nblk == 32:
                perm_copy("dve", x2p, x2n, True)
                S.dma(g.X2P[:, b, :], x2p, q="pool")
        barrier(g)
    g.cur = st0
    if dbg == "hy_a":
        return UT
    (long_conv or toeplitz_conv)(0)
    if dbg == "hy_b":
        return UT
    with ExitStack() as sc:
        g.cur = sc
        x1t = [sb("x1t%d" % i, [128, 512], BF16) for i in range(2)]
        zn = sb("zn", [128, L], BF16)
        zp = sb("zp", [128, L], BF16)
        for b in range(2):
            for bi in range(32):
                t0 = bi * 512
                xt_ = x1t[bi % 2]
                S.dma(xt_, g.X1[:, b, t0:t0 + 512], q="sp")
                P = ps[bi % 2]
                for sbk in range(4):
                    jb = ut_col(bi * 4 + sbk, b)
                    S.op("pe", lambda e: e.matmul(P.ap[:, sbk * 128:(sbk + 1) * 128], lhsT=UT.ap[:, :, jb], rhs=Jmat.ap, start=True, stop=True),
                         r=UTc + [Jmat], w=[P], inc=(sbk == 3))
                S.op("dve", lambda e: e.tensor_tensor(out=zn.ap[:, t0:t0 + 512], in0=P.ap, in1=xt_.ap, op=ALU.mult), r=[P, xt_], w=[zn])
            perm_copy("dve", zp, zn, True)
            S.dma(g.Z1P[:, b, :], zp, q="pool")
        barrier(g)
        zc = [sb("zc%d" % i, [128, 512], BF16) for i in range(2)]
        for b in range(2):
            for bi in range(32):
                t0 = bi * 512
                z_ = zc[bi % 2]
                S.dma(z_, g.Z1P[:, b, t0:t0 + 512], q="sp")
                P = ps[2 + bi % 2]
                pb = P.ap.bitcast(BF16)
                for sbk in range(4):
                    S.op("pe", lambda e: e.transpose(out=pb[:, sbk * 128:(sbk + 1) * 128], in_=z_.ap[:, sbk * 128:(sbk + 1) * 128],
                                                     identity=g.identb.ap), r=[z_, g.identb], w=[P], inc=(sbk == 3))
                uo, ur = ut_blk4(bi * 4, b)
                S.op("act", lambda e: e.copy(out=uo, in_=ur(pb[:, 0:512])), r=[P], w=UTc)
        barrier(g)
    g.cur = st0
    (long_conv or toeplitz_conv)(1)
    with ExitStack() as sc:
        g.cur = sc
        x2t = [sb("x2t%d" % i, [128, 512], BF16) for i in range(2)]
        opn = sb("opn", [128, L], BF16)
        onat = sb("onat", [128, L], BF16)
        gf = [sb("gf%d" % i, [128, 512], F32) for i in range(2)]
        sqh = [sb("sqh%d" % i, [128, 512], BF16) for i in range(2)]
        rsh = [sb("rsh%d" % i, [128, 512], F32) for i in range(2)]
        for b in range(2):
            for bi in range(32):
                t0 = bi * 512
                a = bi % 2
                S.dma(x2t[a], g.X2P[:, b, t0:t0 + 512], q="sp")
                P = ps[a]
                for sbk in range(4):
                    jb = ut_col(bi * 4 + sbk, b)
                    S.op("pe", lambda e: e.matmul(P.ap[:, sbk * 128:(sbk + 1) * 128], lhsT=UT.ap[:, :, jb], rhs=Jmat.ap, start=True, stop=True),
                         r=UTc + [Jmat], w=[P], inc=(sbk == 3))
                S.op("dve", lambda e: e.tensor_tensor(out=gf[a].ap, in0=P.ap, in1=x2t[a].ap, op=ALU.mult), r=[P, x2t[a]], w=[gf[a]])
                S.op("act", lambda e: e.activation(out=sqh[a].ap, in_=gf[a].ap, func=AF.Square), r=[gf[a]], w=[sqh[a]])
                PM = ps[2 + a]
                S.op("pe", lambda e: e.matmul(PM.ap, lhsT=blk64.ap, rhs=sqh[a].ap, start=True, stop=True), r=[blk64, sqh[a]], w=[PM])
                S.op("act", lambda e: e.activation(out=rsh[a].ap, in_=PM.ap, func=AF.Ln, bias=g.epsb.ap[:, 0:1], scale=1.0),
                     r=[PM, g.epsb], w=[rsh[a]])
                S.op("act", lambda e: e.activation(out=rsh[a].ap, in_=rsh[a].ap, func=AF.Exp, scale=-0.5), r=[rsh[a]], w=[rsh[a]])
                S.op("dve", lambda e: e.scalar_tensor_tensor(out=opn.ap[:, t0:t0 + 512], in0=gf[a].ap, scalar=hycol.ap[:, 14:15],
                                                             in1=rsh[a].ap, op0=ALU.mult, op1=ALU.mult),
                     r=[gf[a], hycol, rsh[a]], w=[opn])
            perm_copy("dve", onat, opn, False)
            S.dma(g.OHY[:, b, :], onat, q="pool")
        barrier(g)
    g.cur = st0
    return UT


NPB = 24


def collective_allgather(g, xin_ap, xg, reads):
    S, nc = g.S, g.nc
    S._wait("pool", S._deps([_t(r) for r in reads], [xg]))
    ins = nc.gpsimd.collective_compute("AllGather", ALU.bypass, replica_groups=[list(range(NCORE))],
                                       ins=[xin_ap.opt()], outs=[xg.ap.opt()])
    ins.then_inc(S.sem["cc"])
    S.cnt["cc"] += 1
    S._mark(("cc", S.cnt["cc"]), [_t(r) for r in reads], [xg])


def phase_b1(g):
    S, I, ps, nc, sb, st0 = g.S, g.I, g.ps, g.nc, g.sb, g.cur
    g.H1 = g.dram_tmp("H1", [4096, D], F32)
    g.FT = g.dram_tmp("FT", [128, 8, 4096], BF16)
    g.EGT = g.dram_tmp("EGT", [128, 3, 4096], F32)
    g.UB = g.dram_tmp("UB", [128, 8, 128, 128], BF16)
    g.VB = g.dram_tmp("VB", [128, 128, D], BF16)
    pb = sb("pbcol", [128, NPB], F32)
    S.dma(pb, I.pbcol)
    iota = g.iota
    modr = g.modr
    fnr = g.fnr
    S.dma(fnr, I.final_norm.v(I.final_norm.ap.partition_broadcast(128)))
    scb = ExitStack()
    g.cur = scb
    wo = sb("wo", [128, 16, D], BF16)
    for kc in range(16):
        S.dma(wo[:, kc, :], I.w_out[kc * 128:(kc + 1) * 128, :], q="pool")
    Wc = sb("Wc", [128, 8, 2048], BF16)
    G2 = sb("G2row", [128, D], F32)
    with ExitStack() as sc:
        g.cur = sc
        ucv = [sb("ucv%d" % i, [128, 16, 128], BF16) for i in range(2)]
        n = 0
        for kc in range(8):
            for e2c in range(8):
                t_ = ucv[n % 2]
                n += 1
                S.dma(t_, I.u2[kc * 128:(kc + 1) * 128, e2c * 16:(e2c + 1) * 16, :], q="pool")
                S.dma(g.UB[:, kc, e2c * 16:(e2c + 1) * 16, :], t_, q="sp")
        vcv = [sb("vcv%d" % i, [128, 4, D], BF16) for i in range(2)]
        for e2c in range(32):
            t_ = vcv[e2c % 2]
            S.dma(t_, I.v2[:, e2c * 4:(e2c + 1) * 4, :], q="pool")
            S.dma(g.VB[:, e2c * 4:(e2c + 1) * 4, :], t_, q="sp")
        skT = sb("skT", [128, 2, 128], F32)
        S.dma(skT, I.skT)
        wqt = [sb("wqt%d" % i, [128, D], F32) for i in range(2)]
        for hs in range(16):
            w_ = wqt[hs % 2]
            S.dma(w_, I.wqT[hs * 128:(hs + 1) * 128, :], q="sp")
            for db in range(8):
                P = ps[db // 4]
                S.op("pe", lambda e: e.matmul(P.ap[:, (db % 4) * 128:(db % 4 + 1) * 128], lhsT=w_.ap[:, db * 128:(db + 1) * 128],
                                              rhs=skT.ap[:, hs % 2, :], start=True, stop=True), r=[w_, skT], w=[P])
            for half in range(2):
                S.op("dve", lambda e: e.tensor_copy(out=Wc.ap[:, half * 4:(half + 1) * 4, hs * 128:(hs + 1) * 128],
                                                    in_=ps[half].ap.rearrange("p (j n) -> p j n", n=128)), r=[ps[half]], w=[Wc])
        scbc = sb("scbc", [128, 8, 128], F32)
        for kc in range(8):
            S.op("dve", lambda e: e.tensor_copy(out=scbc.ap[:, kc, :], in_=g.scT.ap[:, kc, 0:1].to_broadcast([128, 128])),
                 r=[g.scT], w=[scbc])
        wmr = [sb("wmr%d" % i, [128, 8, 512], F32) for i in range(2)]
        bmr = [sb("bmr%d" % i, [128, 512], F32) for i in range(2)]
        mflat = modr.ap.rearrange("p j d -> p (j d)")
        for cb in range(8):
            cols = slice(2 * D + cb * 512, 2 * D + (cb + 1) * 512)
            w_ = wmr[cb % 2]
            S.dma(w_, I.w_mod.v(I.w_mod.ap[:, cols].rearrange("(kc p) n -> p kc n", p=128)), q="sp")
            S.dma(bmr[cb % 2], I.b_mod.v(I.b_mod.ap[cols].partition_broadcast(128)), q="sp")
            P = ps[2 + cb % 2]
            for kc in range(8):
                S.op("pe", lambda e: e.matmul(P.ap, lhsT=scbc.ap[:, kc, :], rhs=w_.ap[:, kc, :], start=(kc == 0), stop=(kc == 7)),
                     r=[scbc, w_], w=[P], inc=(kc == 7))
            S.op("dve", lambda e: e.tensor_tensor(out=mflat[:, cb * 512:(cb + 1) * 512], in0=P.ap, in1=bmr[cb % 2].ap, op=ALU.add),
                 r=[P, bmr[cb % 2]], w=[modr])
        n2r = sb("n2r", [128, D], F32)
        S.dma(n2r, I.norm2.v(I.norm2.ap.partition_broadcast(128)))
        S.op("dve", lambda e: e.scalar_tensor_tensor(out=G2.ap, in0=modr.ap[:, 2, :], scalar=1.0, in1=n2r.ap, op0=ALU.add, op1=ALU.mult),
             r=[modr, n2r], w=[G2])
        barrier(g)
    g.cur = scb
    cand = [sb("cand%d" % i, [128, 512], BF16) for i in range(4)]
    oT = sb("oT", [128, 16, 512], BF16)
    oTk = [T(oT.ap[:, kc, :], "oT%d" % kc) for kc in range(16)]
    xt = [sb("bxt%d" % i, [128, D], F32) for i in range(2)]
    h1 = [sb("bh1%d" % i, [128, D], F32) for i in range(2)]
    tmpf = sb("btmpf", [128, D], F32)
    fbt = sb("bfb", [128, D], BF16)
    junk = sb("bjunk", [128, D], BF16)
    ssq = sb("bssq", [128, 1], F32)
    fT = [sb("bfT%d" % i, [128, 8, 128], BF16) for i in range(2)]
    s_sb = sb("s_sb", [128, 16, 128], F32)
    tmpS = sb("tmpS", [128, 128], F32)
    v16 = sb("v16", [128, 16, 16], F32)
    i16 = sb("i16", [128, 16, 16], U32)
    i16f = sb("i16f", [128, 16, 16], F32)
    candv = sb("candv", [128, 8, 256], F32)
    tmpC = sb("tmpC", [128, 256], F32)
    sc16 = sb("sc16", [128, 8, 16], F32)
    ci = sb("ci", [128, 8, 16], U32)
    iku = sb("iku", [128, 8, 16], U32)
    ikf = [sb("ikf%d" % i, [128, 8, 16], F32) for i in range(2)]
    oh = sb("oh", [128, 8, 16, 16], F32)
    EG = sb("EG", [128, 3, 128], F32)
    ex = sb("ex", [128, 8, 16], F32)
    sm = sb("sm", [128, 8], F32)
    egt = [sb("egt%d" % i, [128, 3, 128], F32) for i in range(2)]
    ncand = 0
    for grp in range(8):
        for kc in range(16):
            for q in range(8):
                hb, tb = q // 4, q % 4
                if kc < 8:
                    row0 = 512 * (4 * hb + kc // 2) + 128 * (kc % 2)
                    mcol = q
                else:
                    kk = kc - 8
                    row0 = 512 * kk + 256 + 128 * hb
                    mcol = (8 if kk < 4 else 16) + q
                c_ = cand[ncand % 4]
                ncand += 1
                S.dma(c_, g.XG[row0:row0 + 128, 4096 * tb + 512 * grp: 4096 * tb + 512 * (grp + 1)], q=("sp" if ncand % 2 else "pool"))
                if q == 0:
                    S.op("dve", lambda e: e.tensor_scalar(out=oTk[kc].ap, in0=c_.ap, scalar1=pb.ap[:, mcol:mcol + 1], scalar2=None,
                                                          op0=ALU.mult), r=[c_, pb], w=[oTk[kc]])
                else:
                    S.op("dve", lambda e: e.scalar_tensor_tensor(out=oTk[kc].ap, in0=c_.ap, scalar=pb.ap[:, mcol:mcol + 1], in1=oTk[kc].ap,
                                                                 op0=ALU.mult, op1=ALU.add), r=[c_, pb, oTk[kc]], w=[oTk[kc]])
        for tl in range(4):
            ti = grp * 4 + tl
            a = ti % 2
            S.dma(xt[a], I.x_res[ti * 128:(ti + 1) * 128, :], q="sp")
            for half in range(2):
                P = ps[half]
                for kc in range(16):
                    S.op("pe", lambda e: e.matmul(P.ap, lhsT=oTk[kc].ap[:, tl * 128:(tl + 1) * 128], rhs=wo.ap[:, kc, half * 512:(half + 1) * 512],
                                                  start=(kc == 0), stop=(kc == 15)), r=[oTk[kc], wo], w=[P], inc=(kc == 15))
                S.op("dve", lambda e: e.tensor_tensor(out=tmpf.ap[:, half * 512:(half + 1) * 512], in0=P.ap,
                                                      in1=modr.ap[:, 0, half * 512:(half + 1) * 512], op=ALU.mult), r=[P, modr], w=[tmpf])
            S.op("dve", lambda e: e.tensor_tensor(out=h1[a].ap, in0=tmpf.ap, in1=xt[a].ap, op=ALU.add), r=[tmpf, xt[a]], w=[h1[a]])
            S.dma(g.H1[ti * 128:(ti + 1) * 128, :], h1[a], q="pool")
            S.op("act", lambda e: e.activation(out=junk.ap, in_=h1[a].ap, func=AF.Square, accum_out=ssq.ap), r=[h1[a]], w=[junk, ssq])
            S.op("act", lambda e: e.activation(out=ssq.ap, in_=ssq.ap, func=AF.Ln, bias=g.epsb.ap[:, 0:1], scale=1.0 / D),
                 r=[ssq, g.epsb], w=[ssq])
            S.op("act", lambda e: e.activation(out=ssq.ap, in_=ssq.ap, func=AF.Exp, scale=-0.5), r=[ssq], w=[ssq])
            S.op("dve", lambda e: e.scalar_tensor_tensor(out=tmpf.ap, in0=h1[a].ap, scalar=ssq.ap[:, 0:1], in1=G2.ap, op0=ALU.mult,
                                                         op1=ALU.mult), r=[h1[a], ssq, G2], w=[tmpf])
            S.op("dve", lambda e: e.tensor_tensor(out=fbt.ap, in0=tmpf.ap, in1=modr.ap[:, 1, :], op=ALU.add), r=[tmpf, modr], w=[fbt])
            pbf = ps[2].ap.bitcast(BF16)
            for kc in range(8):
                S.op("pe", lambda e: e.transpose(out=pbf[:, kc * 128:(kc + 1) * 128], in_=fbt.ap[:, kc * 128:(kc + 1) * 128],
                                                 identity=g.identb.ap), r=[fbt, g.identb], w=[ps[2]], inc=(kc == 7))
            S.op("act", lambda e: e.copy(out=fT[a].ap, in_=pbf.rearrange("p (k t) -> p k t", t=128)), r=[ps[2]], w=[fT[a]])
            S.dma(g.FT[:, :, ti * 128:(ti + 1) * 128], fT[a], q="pool")
            for nb in range(4):
                P = ps[3 + nb]
                for kc in range(8):
                    S.op("pe", lambda e: e.matmul(P.ap, lhsT=fT[a].ap[:, kc, :], rhs=Wc.ap[:, kc, nb * 512:(nb + 1) * 512],
                                                  start=(kc == 0), stop=(kc == 7)), r=[fT[a], Wc], w=[P], inc=(kc == 7))
                if nb % 2 == 0:
                    S.op("act", lambda e: e.copy(out=s_sb.ap[:, nb * 4:(nb + 1) * 4, :], in_=P.ap.rearrange("p (j n) -> p j n", n=128)),
                         r=[P], w=[s_sb])
                else:
                    S.op("dve", lambda e: e.tensor_copy(out=s_sb.ap[:, nb * 4:(nb + 1) * 4, :], in_=P.ap.rearrange("p (j n) -> p j n", n=128)),
                         r=[P], w=[s_sb])
            for hs in range(16):
                S.op("dve", lambda e: e.max(out=v16.ap[:, hs, 0:8], in_=s_sb.ap[:, hs, :]), r=[s_sb], w=[v16])
                S.op("dve", lambda e: e.max_index(out=i16.ap[:, hs, 0:8], in_max=v16.ap[:, hs, 0:8], in_values=s_sb.ap[:, hs, :]),
                     r=[s_sb, v16], w=[i16])
                S.op("dve", lambda e: e.match_replace(out=tmpS.ap, in_to_replace=v16.ap[:, hs, 0:8], in_values=s_sb.ap[:, hs, :],
                                                      imm_value=-1e30), r=[s_sb, v16], w=[tmpS])
                S.op("dve", lambda e: e.max(out=v16.ap[:, hs, 8:16], in_=tmpS.ap), r=[tmpS], w=[v16])
                S.op("dve", lambda e: e.max_index(out=i16.ap[:, hs, 8:16], in_max=v16.ap[:, hs, 8:16], in_values=tmpS.ap),
                     r=[tmpS, v16], w=[i16])
            S.op("dve", lambda e: e.tensor_copy(out=i16f.ap, in_=i16.ap), r=[i16], w=[i16f])
            v4 = v16.ap.rearrange("p (h s) k -> p h s k", s=2)
            S.op("dve", lambda e: e.tensor_tensor(out=candv.ap.rearrange("p h (i j) -> p h i j", j=16),
                                                  in0=v4[:, :, 0, :].unsqueeze(3).to_broadcast([128, 8, 16, 16]),
                                                  in1=v4[:, :, 1, :].unsqueeze(2).to_broadcast([128, 8, 16, 16]), op=ALU.add),
                 r=[v16], w=[candv])
            for h in range(8):
                S.op("dve", lambda e: e.max(out=sc16.ap[:, h, 0:8], in_=candv.ap[:, h, :]), r=[candv], w=[sc16])
                S.op("dve", lambda e: e.max_index(out=ci.ap[:, h, 0:8], in_max=sc16.ap[:, h, 0:8], in_values=candv.ap[:, h, :]),
                     r=[candv, sc16], w=[ci])
                S.op("dve", lambda e: e.match_replace(out=tmpC.ap, in_to_replace=sc16.ap[:, h, 0:8], in_values=candv.ap[:, h, :],
                                                      imm_value=-1e30), r=[candv, sc16], w=[tmpC])
                S.op("dve", lambda e: e.max(out=sc16.ap[:, h, 8:16], in_=tmpC.ap), r=[tmpC], w=[sc16])
                S.op("dve", lambda e: e.max_index(out=ci.ap[:, h, 8:16], in_max=sc16.ap[:, h, 8:16], in_values=tmpC.ap),
                     r=[tmpC, sc16], w=[ci])
            i4 = i16f.ap.rearrange("p (h s) k -> p h s k", s=2)
            for side in range(2):
                if side == 0:
                    S.op("dve", lambda e: e.tensor_single_scalar(out=iku.ap, in_=ci.ap, scalar=4, op=ALU.logical_shift_right), r=[ci], w=[iku])
                else:
                    S.op("dve", lambda e: e.tensor_single_scalar(out=iku.ap, in_=ci.ap, scalar=15, op=ALU.bitwise_and), r=[ci], w=[iku])
                S.op("dve", lambda e: e.tensor_copy(out=ikf[side].ap, in_=iku.ap), r=[iku], w=[ikf[side]])
                S.op("dve", lambda e: e.tensor_tensor(out=oh.ap, in0=iota.ap[:, 0:16].unsqueeze(1).unsqueeze(1).to_broadcast([128, 8, 16, 16]),
                                                      in1=ikf[side].ap.unsqueeze(3).to_broadcast([128, 8, 16, 16]), op=ALU.is_equal),
                     r=[iota, ikf[side]], w=[oh])
                S.op("dve", lambda e: e.tensor_tensor(out=oh.ap, in0=oh.ap, in1=i4[:, :, side, :].unsqueeze(2).to_broadcast([128, 8, 16, 16]),
                                                      op=ALU.mult), r=[oh, i16f], w=[oh])
                S.op("dve", lambda e: e.tensor_reduce(out=EG.ap[:, side, :].rearrange("p (h k) -> p h k", k=16), in_=oh.ap, axis=AX.X,
                                                      op=ALU.add), r=[oh], w=[EG])
            S.op("dve", lambda e: e.tensor_tensor(out=ex.ap, in0=sc16.ap, in1=sc16.ap[:, :, 0:1].to_broadcast([128, 8, 16]), op=ALU.subtract),
                 r=[sc16], w=[ex])
            S.op("act", lambda e: e.activation(out=ex.ap, in_=ex.ap, func=AF.Exp), r=[ex], w=[ex])
            S.op("dve", lambda e: e.tensor_reduce(out=sm.ap, in_=ex.ap, axis=AX.X, op=ALU.add), r=[ex], w=[sm])
            S.op("dve", lambda e: e.reciprocal(out=sm.ap, in_=sm.ap), r=[sm], w=[sm])
            S.op("dve", lambda e: e.tensor_tensor(out=EG.ap[:, 2, :].rearrange("p (h k) -> p h k", k=16), in0=ex.ap,
                                                  in1=sm.ap.unsqueeze(2).to_broadcast([128, 8, 16]), op=ALU.mult), r=[ex, sm], w=[EG])
            for j in range(3):
                S.op("pe", lambda e: e.transpose(out=ps[7].ap[:, j * 128:(j + 1) * 128], in_=EG.ap[:, j, :], identity=g.ident.ap),
                     r=[EG, g.ident], w=[ps[7]], inc=(j == 2))
            S.op("act", lambda e: e.copy(out=egt[a].ap, in_=ps[7].ap[:, 0:384].rearrange("p (j t) -> p j t", t=128)), r=[ps[7]], w=[egt[a]])
            S.dma(g.EGT[:, :, ti * 128:(ti + 1) * 128], egt[a], q="pool")
    barrier(g)
    scb.close()
    g.cur = st0


def phase_b2(g):
    S, I, ps, nc, sb = g.S, g.I, g.ps, g.nc, g.sb
    out = T(nc.dram_tensor("out", [4096, D], F32, kind="ExternalOutput").ap(), "out")
    iota = g.iota
    Gs = sb("Gs", [128, 128, 256], BF16)
    ub = [sb("ub%d" % i, [128, 8, 4, 128], BF16) for i in range(2)]
    vb = [sb("vb%d" % i, [128, 4, D], BF16) for i in range(2)]
    fTg = sb("fTg", [128, 8, 256], BF16)
    egt = sb("egtg", [128, 3, 256], F32)
    ohL = [sb("ohL%d" % i, [128, 4, 128], F32) for i in range(2)]
    L4 = [sb("L4_%d" % i, [128, 4, 128], BF16) for i in range(2)]
    R4 = [sb("R4_%d" % i, [128, 4, 128], BF16) for i in range(2)]
    A = [sb("Aact%d" % i, [128, 256], BF16) for i in range(2)]
    W = [sb("Wact%d" % i, [128, 256], BF16) for i in range(2)]
    h1t = sb("h1t", [128, D], F32)
    tmpf = sb("ptmpf", [128, D], F32)
    h2 = sb("h2", [128, D], F32)
    junk = sb("pjunk", [128, D], BF16)
    ssq = sb("pssq", [128, 1], F32)
    ot = [sb("pot%d" % i, [128, D], F32) for i in range(2)]
    for grp in range(16):
        t0 = grp * 256
        S.dma(fTg, g.FT[:, :, t0:t0 + 256], q="sp")
        S.dma(egt, g.EGT[:, :, t0:t0 + 256], q="sp")
        for t4 in range(64):
            a = t4 % 2
            ts_ = slice(t4 * 4, t4 * 4 + 4)
            S.op("dve", lambda e: e.tensor_tensor(out=ohL[a].ap, in0=iota.ap.unsqueeze(1).to_broadcast([128, 4, 128]),
                                                  in1=egt.ap[:, 0, ts_].unsqueeze(2).to_broadcast([128, 4, 128]), op=ALU.is_equal),
                 r=[iota, egt], w=[ohL[a]])
            S.op("dve", lambda e: e.tensor_tensor(out=L4[a].ap, in0=ohL[a].ap, in1=egt.ap[:, 2, ts_].unsqueeze(2).to_broadcast([128, 4, 128]),
                                                  op=ALU.mult), r=[ohL[a], egt], w=[L4[a]])
            S.op("dve", lambda e: e.tensor_tensor(out=R4[a].ap, in0=iota.ap.unsqueeze(1).to_broadcast([128, 4, 128]),
                                                   in1=egt.ap[:, 1, ts_].unsqueeze(2).to_broadcast([128, 4, 128]), op=ALU.is_equal),
                 r=[iota, egt], w=[R4[a]])
            PG = ps[4 + a]
            for tl in range(4):
                S.op("pe", lambda e: e.matmul(PG.ap[:, tl * 128:(tl + 1) * 128], lhsT=L4[a].ap[:, tl, :], rhs=R4[a].ap[:, tl, :],
                                              start=True, stop=True), r=[L4[a], R4[a]], w=[PG], inc=(tl == 3))
            if a == 0:
                S.op("act", lambda e: e.copy(out=Gs.ap[:, :, ts_], in_=PG.ap.rearrange("p (t e) -> p e t", e=128)), r=[PG], w=[Gs])
            else:
                S.op("dve", lambda e: e.tensor_copy(out=Gs.ap[:, :, ts_], in_=PG.ap.rearrange("p (t e) -> p e t", e=128)), r=[PG], w=[Gs])
        for e2c in range(32):
            u_ = ub[e2c % 2]
            v_ = vb[e2c % 2]
            S.dma(u_, g.UB[:, :, e2c * 4:(e2c + 1) * 4, :], q="sp")
            S.dma(v_, g.VB[:, e2c * 4:(e2c + 1) * 4, :], q="pool")
            for e2l in range(4):
                e2 = e2c * 4 + e2l
                a = e2 % 2
                PA = ps[6 + a]
                for kc in range(8):
                    S.op("pe", lambda e: e.matmul(PA.ap[:, 0:256], lhsT=u_.ap[:, kc, e2l, :], rhs=fTg.ap[:, kc, :], start=(kc == 0), stop=(kc == 7)),
                         r=[u_, fTg], w=[PA], inc=(kc == 7))
                S.op("act", lambda e: e.activation(out=A[a].ap, in_=PA.ap[:, 0:256], func=AF.Gelu), r=[PA], w=[A[a]])
                S.op(("dve" if a == 0 else "pool"), lambda e: e.tensor_tensor(out=W[a].ap, in0=A[a].ap, in1=Gs.ap[:, e2, :], op=ALU.mult),
                     r=[A[a], Gs], w=[W[a]])
                for tl in range(2):
                    for half in range(2):
                        S.op("pe", lambda e: e.matmul(ps[tl * 2 + half].ap, lhsT=W[a].ap[:, tl * 128:(tl + 1) * 128],
                                                      rhs=v_.ap[:, e2l, half * 512:(half + 1) * 512], start=(e2 == 0), stop=(e2 == 127)),
                             r=[W[a], v_], w=[ps[tl * 2 + half]], inc=(e2 == 127 or (tl == 1 and half == 1)))
        for tl in range(2):
            ti = grp * 2 + tl
            S.dma(h1t, g.H1[ti * 128:(ti + 1) * 128, :], q="sp")
            for half in range(2):
                S.op("dve", lambda e: e.tensor_tensor(out=tmpf.ap[:, half * 512:(half + 1) * 512], in0=ps[tl * 2 + half].ap,
                                                      in1=g.modr.ap[:, 3, half * 512:(half + 1) * 512], op=ALU.mult),
                     r=[ps[tl * 2 + half], g.modr], w=[tmpf])
            S.op("dve", lambda e: e.tensor_tensor(out=h2.ap, in0=tmpf.ap, in1=h1t.ap, op=ALU.add), r=[tmpf, h1t], w=[h2])
            S.op("act", lambda e: e.activation(out=junk.ap, in_=h2.ap, func=AF.Square, accum_out=ssq.ap), r=[h2], w=[junk, ssq])
            S.op("act", lambda e: e.activation(out=ssq.ap, in_=ssq.ap, func=AF.Ln, bias=g.epsb.ap[:, 0:1], scale=1.0 / D),
                 r=[ssq, g.epsb], w=[ssq])
            S.op("act", lambda e: e.activation(out=ssq.ap, in_=ssq.ap, func=AF.Exp, scale=-0.5), r=[ssq], w=[ssq])
            o_ = ot[tl]
            S.op("dve", lambda e: e.scalar_tensor_tensor(out=o_.ap, in0=h2.ap, scalar=ssq.ap[:, 0:1], in1=g.fnr.ap, op0=ALU.mult, op1=ALU.mult),
                 r=[h2, ssq, g.fnr], w=[o_])
            S.dma(out[ti * 128:(ti + 1) * 128, :], o_, q="pool")
    if os.environ.get("BDBG"):
        d1 = T(nc.dram_tensor("dbg_h1", [128, D], F32, kind="ExternalOutput").ap())
        d2 = T(nc.dram_tensor("dbg_ft", [128, 8, 128], BF16, kind="ExternalOutput").ap())
        d3 = T(nc.dram_tensor("dbg_egt", [128, 3, 128], F32, kind="ExternalOutput").ap())
        d4 = T(nc.dram_tensor("dbg_gs", [128, 128, 256], BF16, kind="ExternalOutput").ap())
        S.dma(d1, g.H1[0:128, :]); S.dma(d2, g.FT[:, :, 0:128]); S.dma(d3, g.EGT[:, :, 0:128]); S.dma(d4, Gs)
    S.finish([out])
```
